# Optimizing a Trainium2 kernel written in Bass

```python
import jax
import jax.numpy as jnp
from jax import lax
import numpy as np

D_MODEL = 4096
BATCH = 4
SEQ = 2048
DEPTH = 2

SWA_HEADS = 16
SWA_KV_HEADS = 2
SWA_HEAD_DIM = 64
SWA_WINDOW = 128
SWA_BLOCK = 128
MLSTM_HEADS = 8
MLSTM_QK_DIM = 64
MLSTM_V_DIM = 128
MLSTM_CHUNK = 64
FOX_HEADS = 8
FOX_HEAD_DIM = 128
FOX_BLOCK = 128
D_FF = 4 * D_MODEL
LN_EPS = 1e-5
DN_ALPHA = (2 * DEPTH) ** 0.25
DN_BETA = (8 * DEPTH) ** -0.25

A_Q = SWA_HEADS * SWA_HEAD_DIM
A_KV = SWA_KV_HEADS * SWA_HEAD_DIM
B_QK = MLSTM_HEADS * MLSTM_QK_DIM
B_V = MLSTM_HEADS * MLSTM_V_DIM
C_W = FOX_HEADS * FOX_HEAD_DIM
SEG_WIDTHS = (A_Q, A_KV, A_KV,
              B_QK, B_QK, B_V, MLSTM_HEADS, MLSTM_HEADS, B_V,
              C_W, C_W, C_W, FOX_HEADS,
              D_MODEL, D_MODEL, D_MODEL)
VALUE_SEGS = (2, 5, 11)
D_IN = sum(SEG_WIDTHS)

kernel_name = 'hybrid_swa_mlstm_fox_deepnorm'


def split_columns(z):
    parts, off = [], 0
    for w in SEG_WIDTHS:
        parts.append(z[..., off:off + w])
        off += w
    return parts


def layer_norm(x, g, b):
    xf = x.astype(jnp.float32)
    mu = jnp.mean(xf, axis=-1, keepdims=True)
    var = jnp.mean(jnp.square(xf - mu), axis=-1, keepdims=True)
    y = (xf - mu) * lax.rsqrt(var + LN_EPS) * g.astype(jnp.float32) + b.astype(jnp.float32)
    return y.astype(x.dtype)


def alibi_slopes(n_heads):
    return 2.0 ** (-8.0 * jnp.arange(1, n_heads + 1, dtype=jnp.float32) / n_heads)


def swa_sink_attention(q, k, v, sinks):
    bsz, seq, _, dh = q.shape
    blk = SWA_BLOCK
    nb = seq // blk
    grp = SWA_HEADS // SWA_KV_HEADS
    f32 = jnp.float32
    qb = q.reshape(bsz, nb, blk, SWA_KV_HEADS, grp, dh)
    pad = jnp.zeros((bsz, blk, SWA_KV_HEADS, dh), k.dtype)
    kp = jnp.concatenate([pad, k], axis=1).reshape(bsz, nb + 1, blk, SWA_KV_HEADS, dh)
    vp = jnp.concatenate([pad.astype(v.dtype), v], axis=1).reshape(bsz, nb + 1, blk, SWA_KV_HEADS, dh)
    kb = jnp.concatenate([kp[:, :-1], kp[:, 1:]], axis=2)
    vb = jnp.concatenate([vp[:, :-1], vp[:, 1:]], axis=2)
    s = jnp.einsum('bnqhgd,bnkhd->bnhgqk', qb, kb, preferred_element_type=f32) * (dh ** -0.5)
    q_pos = jnp.arange(nb)[:, None] * blk + jnp.arange(blk)[None, :]
    k_pos = jnp.arange(nb)[:, None] * blk - blk + jnp.arange(2 * blk)[None, :]
    dist = q_pos[:, :, None] - k_pos[:, None, :]
    valid = (dist >= 0) & (dist < SWA_WINDOW) & (k_pos[:, None, :] >= 0)
    slopes = alibi_slopes(SWA_HEADS).reshape(SWA_KV_HEADS, grp)
    s = s - slopes[None, None, :, :, None, None] * dist.astype(f32)[None, :, None, None, :, :]
    s = jnp.where(valid[None, :, None, None, :, :], s, -jnp.inf)
    sink = sinks.astype(f32).reshape(SWA_KV_HEADS, grp)[None, None, :, :, None, None]
    m = jnp.maximum(jnp.max(s, axis=-1, keepdims=True), sink)
    p = jnp.exp(s - m)
    p = p / (jnp.sum(p, axis=-1, keepdims=True) + jnp.exp(sink - m))
    o = jnp.einsum('bnhgqk,bnkhd->bnqhgd', p.astype(v.dtype), vb)
    return o.reshape(bsz, seq, SWA_HEADS * dh)


def mlstm_chunkwise(q, k, v, i_pre, f_pre):
    bsz, seq, nh, dk = q.shape
    dv = v.shape[-1]
    L = MLSTM_CHUNK
    nc = seq // L
    f32 = jnp.float32

    def chunks(t):
        t = t.astype(f32).reshape((bsz, nc, L, nh) + t.shape[3:])
        return jnp.moveaxis(t, (1, 3), (0, 2))

    qc = chunks(q)
    kc = chunks(k) * (dk ** -0.5)
    vc = chunks(v)
    ic = chunks(i_pre)
    lfc = chunks(jax.nn.log_sigmoid(f_pre.astype(f32)))
    causal = jnp.tril(jnp.ones((L, L), dtype=bool))

    def step(carry, inp):
        C, n, m = carry
        qq, kk, vv, ig, lf = inp
        b = jnp.cumsum(lf, axis=-1)
        D = b[..., :, None] - b[..., None, :] + ig[..., None, :]
        D = jnp.where(causal, D, -jnp.inf)
        inter = b + m[..., None]
        m_t = jnp.maximum(inter, jnp.max(D, axis=-1))
        w_inter = jnp.exp(inter - m_t)
        S = jnp.einsum('bhtd,bhsd->bhts', qq, kk) * jnp.exp(D - m_t[..., None])
        num = w_inter[..., None] * jnp.einsum('bhtd,bhde->bhte', qq, C) + jnp.einsum('bhts,bhse->bhte', S, vv)
        den = w_inter * jnp.einsum('bhtd,bhd->bht', qq, n) + jnp.sum(S, axis=-1)
        h = num / jnp.maximum(jnp.abs(den), jnp.exp(-m_t))[..., None]
        m_new = m_t[..., -1]
        decay = jnp.exp(b[..., -1] + m - m_new)
        wk = jnp.exp(b[..., -1:] - b + ig - m_new[..., None])
        kw = kk * wk[..., None]
        C_new = decay[..., None, None] * C + jnp.einsum('bhsd,bhse->bhde', kw, vv)
        n_new = decay[..., None] * n + jnp.sum(kw, axis=2)
        return (C_new, n_new, m_new), h

    init = (jnp.zeros((bsz, nh, dk, dv), f32), jnp.zeros((bsz, nh, dk), f32), jnp.zeros((bsz, nh), f32))
    _, h = lax.scan(step, init, (qc, kc, vc, ic, lfc))
    return jnp.moveaxis(h, (0, 2), (1, 3)).reshape(bsz, seq, nh * dv)


def forgetting_attention(q, k, v, f_pre):
    bsz, seq, nh, dh = q.shape
    blk = FOX_BLOCK
    nb = seq // blk
    f32 = jnp.float32
    cum = jnp.cumsum(jax.nn.log_sigmoid(f_pre.astype(f32)), axis=1)
    cum_k = jnp.transpose(cum, (0, 2, 1))
    kh = jnp.transpose(k, (0, 2, 1, 3))
    vh = jnp.transpose(v, (0, 2, 1, 3))
    qb = jnp.transpose(q.reshape(bsz, nb, blk, nh, dh), (1, 0, 3, 2, 4))
    cq = jnp.transpose(cum.reshape(bsz, nb, blk, nh), (1, 0, 3, 2))
    k_pos = jnp.arange(seq)

    def block(args):
        qi, ci, idx = args
        q_pos = idx * blk + jnp.arange(blk)
        s = jnp.einsum('bhqd,bhkd->bhqk', qi, kh, preferred_element_type=f32) * (dh ** -0.5)
        s = s + ci[..., :, None] - cum_k[..., None, :]
        s = jnp.where(k_pos[None, :] <= q_pos[:, None], s, -jnp.inf)
        p = jax.nn.softmax(s, axis=-1)
        return jnp.einsum('bhqk,bhkd->bhqd', p.astype(v.dtype), vh)

    o = lax.map(block, (qb, cq, jnp.arange(nb)))
    return jnp.transpose(o, (1, 0, 3, 2, 4)).reshape(bsz, seq, nh * dh)


def setup_inputs(seed: int = 0) -> dict:
    key = jax.random.key(seed)
    ks = jax.random.split(key, 16)
    f32 = jnp.float32
    nrm = jax.random.normal
    x = nrm(ks[0], (BATCH, SEQ, D_MODEL), f32)
    col_scale = jnp.concatenate([jnp.full((w,), DN_BETA if i in VALUE_SEGS else 1.0, f32)
                                 for i, w in enumerate(SEG_WIDTHS)])
    w_in = nrm(ks[1], (DEPTH, D_MODEL, D_IN), f32) * (D_MODEL ** -0.5) * col_scale
    b_mlstm_i = 0.1 * nrm(ks[2], (DEPTH, MLSTM_HEADS), f32)
    b_mlstm_f = 3.0 + 0.5 * nrm(ks[3], (DEPTH, MLSTM_HEADS), f32)
    b_fox_f = 3.0 + 0.5 * nrm(ks[4], (DEPTH, FOX_HEADS), f32)
    attn_sinks = 0.5 * nrm(ks[5], (DEPTH, SWA_HEADS), f32)
    w_up_swa = nrm(ks[6], (DEPTH, A_Q, D_MODEL), f32) * (A_Q ** -0.5) * DN_BETA
    w_up_mlstm = nrm(ks[7], (DEPTH, B_V, D_MODEL), f32) * (B_V ** -0.5) * DN_BETA
    w_up_fox = nrm(ks[8], (DEPTH, C_W, D_MODEL), f32) * (C_W ** -0.5) * DN_BETA
    w_o = nrm(ks[9], (DEPTH, D_MODEL, D_MODEL), f32) * (D_MODEL ** -0.5) * DN_BETA
    ln1_g = 1.0 + 0.02 * nrm(ks[10], (DEPTH, D_MODEL), f32)
    ln1_b = 0.02 * nrm(ks[11], (DEPTH, D_MODEL), f32)
    w_ff1 = nrm(ks[12], (DEPTH, D_MODEL, D_FF), f32) * (D_MODEL ** -0.5)
    w_ff2 = nrm(ks[13], (DEPTH, D_FF, D_MODEL), f32) * (D_FF ** -0.5) * DN_BETA
    ln2_g = 1.0 + 0.02 * nrm(ks[14], (DEPTH, D_MODEL), f32)
    ln2_b = 0.02 * nrm(ks[15], (DEPTH, D_MODEL), f32)
    return {'x': x, 'w_in': w_in, 'b_mlstm_i': b_mlstm_i, 'b_mlstm_f': b_mlstm_f,
            'b_fox_f': b_fox_f, 'attn_sinks': attn_sinks, 'w_up_swa': w_up_swa,
            'w_up_mlstm': w_up_mlstm, 'w_up_fox': w_up_fox, 'w_o': w_o,
            'ln1_g': ln1_g, 'ln1_b': ln1_b, 'w_ff1': w_ff1, 'w_ff2': w_ff2,
            'ln2_g': ln2_g, 'ln2_b': ln2_b}


def reference(x, w_in, b_mlstm_i, b_mlstm_f, b_fox_f, attn_sinks, w_up_swa, w_up_mlstm,
              w_up_fox, w_o, ln1_g, ln1_b, w_ff1, w_ff2, ln2_g, ln2_b):
    bsz, seq, _ = x.shape
    f32 = jnp.float32
    for l in range(DEPTH):
        z = x @ w_in[l]
        (a_q, a_k, a_v, m_q, m_k, m_v, m_i, m_f, m_o,
         c_q, c_k, c_v, c_f, g_a, g_b, g_c) = split_columns(z)
        y_a = swa_sink_attention(a_q.reshape(bsz, seq, SWA_HEADS, SWA_HEAD_DIM),
                                 a_k.reshape(bsz, seq, SWA_KV_HEADS, SWA_HEAD_DIM),
                                 a_v.reshape(bsz, seq, SWA_KV_HEADS, SWA_HEAD_DIM),
                                 attn_sinks[l])
        h_b = mlstm_chunkwise(m_q.reshape(bsz, seq, MLSTM_HEADS, MLSTM_QK_DIM),
                              m_k.reshape(bsz, seq, MLSTM_HEADS, MLSTM_QK_DIM),
                              m_v.reshape(bsz, seq, MLSTM_HEADS, MLSTM_V_DIM),
                              m_i + b_mlstm_i[l], m_f + b_mlstm_f[l])
        y_b = (jax.nn.sigmoid(m_o.astype(f32)) * h_b).astype(x.dtype)
        y_c = forgetting_attention(c_q.reshape(bsz, seq, FOX_HEADS, FOX_HEAD_DIM),
                                   c_k.reshape(bsz, seq, FOX_HEADS, FOX_HEAD_DIM),
                                   c_v.reshape(bsz, seq, FOX_HEADS, FOX_HEAD_DIM),
                                   c_f + b_fox_f[l])
        mix = (jax.nn.sigmoid(g_a) * (y_a @ w_up_swa[l])
               + jax.nn.sigmoid(g_b) * (y_b @ w_up_mlstm[l])
               + jax.nn.sigmoid(g_c) * (y_c @ w_up_fox[l]))
        x = layer_norm(DN_ALPHA * x + mix @ w_o[l], ln1_g[l], ln1_b[l])
        hid = jnp.square(jax.nn.relu(x @ w_ff1[l]))
        x = layer_norm(DN_ALPHA * x + hid @ w_ff2[l], ln2_g[l], ln2_b[l])
    return x
```

```python
import numpy as np
from contextlib import ExitStack
import concourse.bass as bass
import concourse.mybir as mybir
from concourse.bass_utils import run_bass_kernel_spmd

F32 = mybir.dt.float32
BF16 = mybir.dt.bfloat16
AF = mybir.ActivationFunctionType
ALU = mybir.AluOpType


class Cfg:
    def __init__(self, D=4096, T=2048, TH=1024, HA=16, HKV=2, HB=8, HC=8, DFF=16384, L=2, NCORES=8):
        self.D, self.T, self.TH, self.HA, self.HKV, self.HB, self.HC, self.DFF, self.L = D, T, TH, HA, HKV, HB, HC, DFF, L
        self.NCORES = NCORES
        self.GRP = HA // HKV
        self.A_Q, self.A_KV = HA * 64, HKV * 64
        self.B_QK, self.B_V, self.C_W = HB * 64, HB * 128, HC * 128
        widths = [self.A_Q, self.A_KV, self.A_KV, self.B_QK, self.B_QK, self.B_V, HB, HB, self.B_V,
                  self.C_W, self.C_W, self.C_W, HC, D, D, D]
        names = ['aq', 'ak', 'av', 'mq', 'mk', 'mv', 'mi', 'mf', 'mo', 'cq', 'ck', 'cv', 'cf', 'ga', 'gb', 'gc']
        self.off = {}
        o = 0
        for n, w in zip(names, widths):
            self.off[n] = (o, w)
            o += w
        self.D_IN = o
        self.DC = D // 128
        self.NB = T // 128
        self.NH = T // TH
        self.alpha = float((2 * L) ** 0.25)
        assert self.A_KV == 128 and TH % 512 == 0 and D % 512 == 0


class Buf:
    __slots__ = ("w", "wx", "r")

    def __init__(self):
        self.w = {}
        self.wx = {}
        self.r = {}


def _merge(d, s):
    for k, v in s.items():
        if d.get(k, 0) < v:
            d[k] = v


class KB:
    def __init__(self, nc, es):
        self.nc = nc
        self.es = es
        self.eng = {'pe': nc.tensor, 'act': nc.scalar, 'dve': nc.vector, 'pool': nc.gpsimd, 'sp': nc.sync}
        self.sems = {}
        self.cnt = {}
        for e in self.eng:
            self.sems[e] = es.enter_context(nc.semaphore("s_" + e))
            self.cnt[e] = 0
        self.seen = {e: {} for e in self.eng}
        self.rings = {}
        for q, n in (('sp', 12), ('pool', 6), ('act', 4)):
            self.rings[q] = [[es.enter_context(nc.semaphore(f"d_{q}{i}")), 0] for i in range(n)]
        self.ridx = {q: 0 for q in self.rings}
        self.semobj = dict(self.sems)
        for q, ring in self.rings.items():
            for i, (s, _) in enumerate(ring):
                self.semobj[f"d_{q}{i}"] = s

    def _waits(self, e, deps):
        eng = self.eng[e]
        seen = self.seen[e]
        for k, v in deps.items():
            if seen.get(k, 0) < v:
                eng.wait_ge(self.semobj[k], v)
                seen[k] = v

    def _deps(self, reads, writes, awrites):
        deps = {}
        for b in reads:
            _merge(deps, b.w)
        for b in writes:
            _merge(deps, b.w)
            _merge(deps, b.r)
        for b in awrites:
            _merge(deps, b.wx)
            _merge(deps, b.r)
        return deps

    def _record(self, key, val, reads, writes, awrites):
        for b in reads:
            if b.r.get(key, 0) < val:
                b.r[key] = val
        for b in writes:
            b.w = {key: val}
            b.wx = {key: val}
            b.r = {}
        for b in awrites:
            if b.w.get(key, 0) < val:
                b.w[key] = val

    def op(self, e, fn, reads=(), writes=(), awrites=()):
        self._waits(e, self._deps(reads, writes, awrites))
        ins = fn(self.eng[e])
        ins.then_inc(self.sems[e], 1)
        self.cnt[e] += 1
        self._record(e, self.cnt[e], reads, writes, awrites)

    def dma(self, q, out, in_, reads=(), writes=(), awrites=(), slow=False):
        self._waits(q, self._deps(reads, writes, awrites))
        ring = self.rings[q]
        i = self.ridx[q]
        self.ridx[q] = (i + 1) % len(ring)
        sem, c = ring[i]
        key = f"d_{q}{i}"
        if c > 0:
            self._waits(q, {key: 16 * c})
        (self.eng[q].dma_start(out=out, in_=in_, allow_slow_non_contiguous=True) if slow else self.eng[q].dma_start(out=out, in_=in_)).then_inc(sem, 16)
        ring[i][1] = c + 1
        self._record(key, 16 * (c + 1), reads, writes, awrites)

    def barrier(self):
        allv = {e: self.cnt[e] for e in self.eng if self.cnt[e] > 0}
        for q, ring in self.rings.items():
            for i, (s, c) in enumerate(ring):
                if c > 0:
                    allv[f"d_{q}{i}"] = 16 * c
        for e in self.eng:
            self._waits(e, allv)

    def final_wait(self):
        self.barrier()


def build_program(cfg, debug=False):
    nc = bass.Bass("TRN2", target_bir_lowering=False)
    c = cfg
    D, T, TH, DC, NB, L, DFF = c.D, c.T, c.TH, c.DC, c.NB, c.L, c.DFF
    NTT = TH // 128
    NTG = TH // 512

    def din(name, shape, dt=F32):
        return nc.dram_tensor(name, list(shape), dt, kind="ExternalInput").ap()

    def dscr(name, shape, dt):
        return nc.dram_tensor(name, list(shape), dt, kind=("ExternalOutput" if debug else "Internal")).ap()

    x_in = din("x", [TH, D])
    flags_in = din("flags", [1, 2])
    w_in = [din(f"w_in_{i}", [D, c.D_IN]) for i in range(L)]
    b_i = din("b_mlstm_i", [L, c.HB])
    b_f = din("b_mlstm_f", [L, c.HB])
    b_cf = din("b_fox_f", [L, c.HC])
    sinks = din("attn_sinks", [L, c.HA])
    w_up = [din("w_up_swa", [L, c.A_Q, D]), din("w_up_mlstm", [L, c.B_V, D]), din("w_up_fox", [L, c.C_W, D])]
    w_o = din("w_o", [L, D, D])
    ln1_g = din("ln1_g", [L, D]); ln1_b = din("ln1_b", [L, D])
    w_ff1 = [din(f"w_ff1_{i}", [D, DFF]) for i in range(L)]; w_ff2 = [din(f"w_ff2_{i}", [DFF, D]) for i in range(L)]
    ln2_g = din("ln2_g", [L, D]); ln2_b = din("ln2_b", [L, D])
    c_ident = din("c_ident", [128, 128])
    c_tri = din("c_tri", [128, 128])
    c_alibi = din("c_alibi", [128, c.HA * 256])
    y_out = nc.dram_tensor("y", [TH, D], F32, kind="ExternalOutput").ap()

    xT_scr = dscr("xT_scr", [D, TH], BF16)
    R_FM = c.A_Q + 256 + 2 * c.B_QK + 2 * c.C_W
    o_qa, o_ka, o_mq = 0, c.A_Q, c.A_Q + 256
    o_mk = o_mq + c.B_QK
    o_cq = o_mk + c.B_QK
    o_ck = o_cq + c.C_W
    C_TM = 128 + c.B_V + c.C_W
    NG = 2 * c.HB + c.HC
    FMs = dscr("FMs", [R_FM, TH], BF16)
    TMs = dscr("TMs", [TH, C_TM], BF16)
    CHF, CHT = 512, 256
    fm_rows = [min(CHF, R_FM - k0) for k0 in range(0, R_FM, CHF)]
    FMg_l = [dscr(f"FMg{k}", [2 * rk, TH], BF16) for k, rk in enumerate(fm_rows)]
    TMg_l = [dscr(f"TMg{k}", [2 * CHT, C_TM], BF16) for k in range(TH // CHT)]

    def fm_g(r, row0, n=128):
        k, i = row0 // CHF, row0 % CHF
        return FMg_l[k][r * fm_rows[k] + i:r * fm_rows[k] + i + n, :]

    def tm_g(jb):
        r, lb = jb // (TH // 128), jb % (TH // 128)
        k, ii = lb // (CHT // 128), (lb % (CHT // 128)) * 128
        return TMg_l[k][r * CHT + ii:r * CHT + ii + 128, :]
    gifs = dscr("gifs", [TH, NG], F32); gifg = dscr("gifg", [2 * TH, NG], F32)
    qaT = FMs[o_qa:o_qa + c.A_Q, :]; kaT = FMs[o_ka:o_ka + 256, :]
    mqT = FMs[o_mq:o_mq + c.B_QK, :]; mkT = FMs[o_mk:o_mk + c.B_QK, :]
    cqT = FMs[o_cq:o_cq + c.C_W, :]; ckT = FMs[o_ck:o_ck + c.C_W, :]
    va = TMs[:, 0:128]; mv = TMs[:, 128:128 + c.B_V]; cv = TMs[:, 128 + c.B_V:C_TM]
    gif = gifs
    moT = dscr("moT", [c.B_V, TH], BF16)
    gT = dscr("gT", [3 * D, TH], BF16)
    yT = dscr("yT", [c.A_Q + c.B_V + c.C_W, T], BF16)
    r_scr = dscr("r_scr", [TH, D], F32)
    x1_scr = dscr("x1_scr", [TH, D], F32)
    x2_scr = dscr("x2_scr", [TH, D], F32)
    hidT = dscr("hidT", [DFF, TH], BF16)
    PAIRS = [[2 * i, 2 * i + 1] for i in range(c.NCORES // 2)]
    dbg = {}

    es = ExitStack()
    with es:
        es.enter_context(nc.allow_low_precision("bf16 matmul operands, fp32 accumulation"))
        kb = KB(nc, es)

        sbctr = [0]

        def sb(name, shape, dt):
            sbctr[0] += 1
            return es2.enter_context(nc.sbuf_tensor(f"{name}_{sbctr[0]}", list(shape), dt))

        es2 = es
        ident = sb("ident", [128, 128], F32)
        tri_f = sb("tri_f", [128, 128], F32)
        tri_b = sb("tri_b", [128, 128], BF16)
        ones_f = sb("ones_f", [128, 128], F32)
        ones_b = sb("ones_b", [128, 128], BF16)
        B_const = Buf()
        psum = [es.enter_context(nc.psum_tensor(f"ps{i}", [128, 512], F32)) for i in range(8)]
        PB = [Buf() for _ in range(8)]

        fl = sb("flags", [128, 2], F32)
        kb.dma('sp', fl[:], bass.AP(flags_in.tensor, flags_in.offset, [[0, 128], [1, 2]]), awrites=[B_const])
        kb.dma('sp', ident[:], c_ident, awrites=[B_const])
        kb.dma('sp', tri_f[:], c_tri, awrites=[B_const])
        kb.op('dve', lambda e: e.tensor_copy(out=tri_b[:], in_=tri_f[:]), reads=[B_const], awrites=[B_const])
        ident_b = sb("ident_b", [128, 128], BF16)
        kb.op('dve', lambda e: e.tensor_copy(out=ident_b[:], in_=ident[:]), reads=[B_const], awrites=[B_const])
        psum_b = [p[:].bitcast(BF16) for p in psum]
        kb.op('dve', lambda e: e.memset(ones_f[:], 1.0), awrites=[B_const])
        kb.op('dve', lambda e: e.memset(ones_b[:], 1.0), awrites=[B_const])
        kb.barrier()

        class TransposeCtx:
            def __init__(self):
                self.i = 0

        tctx = TransposeCtx()

        def transpose_tile(src, src_buf, dst_fn, dst_buf, dst_mode):
            for cq in range(DC // 4):
                bi = tctx.i % 4
                tctx.i += 1
                ps, pb = psum_b[bi], PB[bi]

                def mm(e, cq=cq, ps=ps):
                    ins = None
                    for k in range(4):
                        ins = e.transpose(out=ps[:, k * 128:(k + 1) * 128], in_=src[:, (cq * 4 + k) * 128:(cq * 4 + k + 1) * 128], identity=ident_b[:])
                    return ins
                kb.op('pe', mm, reads=[src_buf, B_const], writes=[pb])
                eng = 'act' if (cq % 2 == 0) else 'dve'
                d = dst_fn(cq)
                if eng == 'act':
                    kb.op('act', lambda e, d=d, ps=ps: e.activation(out=d, in_=ps[:, 0:512].rearrange("p (k t) -> p k t", k=4), func=AF.Copy),
                          reads=[pb], **{dst_mode: [dst_buf]})
                else:
                    kb.op('dve', lambda e, d=d, ps=ps: e.tensor_copy(out=d, in_=ps[:, 0:512].rearrange("p (k t) -> p k t", k=4)),
                          reads=[pb], **{dst_mode: [dst_buf]})

        xT_v = xT_scr.rearrange("(c p) t -> p c t", p=128)
        B_xT = Buf()

        def to_xT_scr(src_dram, B_src):
            with ExitStack() as es_l:
                nonlocal es2
                es2_old = es2
                es2 = es_l
                xt = [sb(f"p0_xt{i}", [128, D], F32) for i in range(2)]
                Bx = [Buf(), Buf()]
                st = [sb(f"p0_st{i}", [128, DC, 128], BF16) for i in range(2)]
                Bs = [Buf(), Buf()]
                xb = [sb(f"p0_xb{i}", [128, D], BF16) for i in range(2)]
                Bxb = [Buf(), Buf()]
                for tt in range(TH // 128):
                    i = tt % 2
                    kb.dma('sp', xt[i][:], src_dram[tt * 128:(tt + 1) * 128, :], reads=[B_src], writes=[Bx[i]])
                    kb.op('act', lambda e, i=i: e.activation(out=xb[i][:], in_=xt[i][:], func=AF.Copy), reads=[Bx[i]], writes=[Bxb[i]])
                    transpose_tile(xb[i], Bxb[i], lambda cq, i=i: st[i][:, cq * 4:(cq + 1) * 4, :], Bs[i], 'awrites')
                    kb.dma('act', xT_v[:, :, tt * 128:(tt + 1) * 128], st[i][:], reads=[Bs[i]], awrites=[B_xT])
                    kb.op('dve', lambda e: e.engine_nop(), writes=[Bs[i]]) if False else None
                    Bs[i].wx = dict(Bs[i].w)
                kb.barrier()
                es2 = es2_old

        B_xin = Buf()
        to_xT_scr(x_in, B_xin)

        def gemm_half(actT, B_act, KC, blocks, wbufs, Bw, wctr):
            for blk in blocks:
                wi = wctr[0] % len(wbufs)
                wctr[0] += 1
                wb, bw = wbufs[wi], Bw[wi]
                loads = blk['wload'](wb)
                first = True
                for (o_ap, i_ap) in loads:
                    if first:
                        kb.dma('pool', o_ap, i_ap, writes=[bw])
                        first = False
                    else:
                        kb.dma('pool', o_ap, i_ap, awrites=[bw])
                if 'before_mm' in blk:
                    blk['before_mm']()
                ncols = blk['ncols']
                if blk['orient'] == 'F':
                    ncc = ncols // 128
                    bidx = 0
                    for cc in range(ncc):
                        for tg in range(NTG):
                            b = blk.get('bank0', 0) + bidx
                            bidx += 1
                            ps, pb = psum[b % 8], PB[b % 8]

                            def mm(e, cc=cc, tg=tg, ps=ps):
                                ins = None
                                for k in range(KC):
                                    ins = e.matmul(ps[:, :], lhsT=wb[:, k, cc * 128:(cc + 1) * 128], rhs=actT[:, k, tg * 512:(tg + 1) * 512],
                                                   start=(k == 0), stop=(k == KC - 1))
                                return ins
                            kb.op('pe', mm, reads=[bw, B_act], writes=[pb])
                            blk['epi'](cc, tg, ps, pb)
                else:
                    for tt in range(NTT):
                        ps, pb = psum[tt % 8], PB[tt % 8]

                        def mm(e, tt=tt, ps=ps):
                            ins = None
                            for k in range(KC):
                                ins = e.matmul(ps[:, :ncols], lhsT=actT[:, k, tt * 128:(tt + 1) * 128], rhs=wb[:, k, :ncols],
                                               start=(k == 0), stop=(k == KC - 1))
                            return ins
                        kb.op('pe', mm, reads=[bw, B_act], writes=[pb])
                        blk['epi'](tt, ps, pb)

        def wview(w2d, k0, kc, c0, ncols):
            return w2d[k0 * 128:(k0 + kc) * 128, c0:c0 + ncols].rearrange("(c p) n -> p c n", p=128)

        class Ring:
            def __init__(self, tiles):
                self.t = tiles
                self.b = [Buf() for _ in tiles]
                self.i = 0

            def next(self):
                i = self.i % len(self.t)
                self.i += 1
                return self.t[i], self.b[i]

        epi_ctr = [0]

        def epi_engine():
            epi_ctr[0] += 1
            return 'act' if epi_ctr[0] % 2 == 0 else 'dve'

        B_w2b = Buf()
        x_res = x_in
        B_xres = B_xin
        for l in range(L):
            B_scr = {n: Buf() for n in ['qaT', 'kaT', 'va', 'mqT', 'mkT', 'mv', 'moT', 'cqT', 'ckT', 'cv', 'gif', 'gT', 'yT', 'r', 'x1', 'x2', 'hid']}
            with ExitStack() as es_l:
                es2 = es_l
                actA = sb("A_act", [128, DC, TH], BF16)
                B_A = Buf()
                wbufs = [sb(f"A_w{i}", [128, DC, 512], BF16) for i in range(2)]
                Bw = [Buf(), Buf()]
                wctr = [0]
                stb = Ring([sb(f"A_sb{i}", [128, 512], BF16) for i in range(6)])
                stf = Ring([sb(f"A_sf{i}", [128, 32], F32) for i in range(4)])
                W = w_in[l]
                for hf in range(1):
                    t0 = 0
                    kb.dma('sp', actA[:], xT_v[:, :, t0:t0 + TH], reads=[B_xT], writes=[B_A])
                    blocks = []

                    def mk_F(seg, dst, func, scale, dst_row0=0, swap=False):
                        o, wd = c.off[seg]
                        for c0 in range(0, wd, 512):
                            ncols = min(512, wd - c0)

                            def wload(wb, o=o, c0=c0, ncols=ncols, swap=swap):
                                if not swap:
                                    return [(wb[:, :, :ncols], wview(W, 0, DC, o + c0, ncols))]
                                return [(wb[:, :, 0:64], wview(W, 0, DC, o + 64, 64)), (wb[:, :, 64:128], wview(W, 0, DC, o, 64))]

                            def epi(cc, tg, ps, pb, c0=c0, dst=dst, func=func, scale=scale, dst_row0=dst_row0, seg=seg):
                                st, bs = stb.next()
                                kb.op('act', lambda e: e.activation(out=st[:, :], in_=ps[:, :], func=func, scale=scale), reads=[pb], writes=[bs])
                                r0 = dst_row0 + c0 + cc * 128
                                kb.dma('sp', dst[0][r0:r0 + 128, t0 + tg * 512:t0 + (tg + 1) * 512], st[:, :], reads=[bs], awrites=[B_scr[dst[1]]])
                            blocks.append(dict(wload=wload, ncols=ncols, orient='F', epi=epi))

                    def mk_T(seg, dst, dcol0, fp32=False, seg2=None):
                        o, wd = c.off[seg]
                        if seg2 is not None:
                            wd += c.off[seg2][1]
                        for c0 in range(0, wd, 512):
                            ncols = min(512, wd - c0)

                            def wload(wb, o=o, c0=c0, ncols=ncols):
                                return [(wb[:, :, :ncols], wview(W, 0, DC, o + c0, ncols))]

                            def epi(tt, ps, pb, c0=c0, ncols=ncols, dst=dst, dcol0=dcol0, fp32=fp32):
                                if fp32:
                                    st, bs = stf.next()
                                else:
                                    st, bs = stb.next()
                                eng = epi_engine()
                                if eng == 'act':
                                    kb.op('act', lambda e: e.activation(out=st[:, :ncols], in_=ps[:, :ncols], func=AF.Copy), reads=[pb], writes=[bs])
                                else:
                                    kb.op('dve', lambda e: e.tensor_copy(out=st[:, :ncols], in_=ps[:, :ncols]), reads=[pb], writes=[bs])
                                kb.dma('sp', dst[0][t0 + tt * 128:t0 + (tt + 1) * 128, dcol0 + c0:dcol0 + c0 + ncols], st[:, :ncols],
                                       reads=[bs], awrites=[B_scr[dst[1]]])
                            blocks.append(dict(wload=wload, ncols=ncols, orient='T', epi=epi))

                    mk_F('aq', (qaT, 'qaT'), AF.Copy, 0.125)
                    mk_F('ak', (kaT, 'kaT'), AF.Copy, 1.0)
                    mk_F('ak', (kaT, 'kaT'), AF.Copy, 1.0, dst_row0=128, swap=True)
                    mk_T('av', (va, 'va'), 0)
                    mk_F('mq', (mqT, 'mqT'), AF.Copy, 1.0)
                    mk_F('mk', (mkT, 'mkT'), AF.Copy, 0.125)
                    mk_T('mv', (mv, 'mv'), 0)
                    mk_T('mi', (gif, 'gif'), 0, fp32=True, seg2='mf')
                    mk_F('cq', (cqT, 'cqT'), AF.Copy, float(128 ** -0.5))
                    mk_F('ck', (ckT, 'ckT'), AF.Copy, 1.0)
                    mk_T('cv', (cv, 'cv'), 0)
                    mk_T('cf', (gif, 'gif'), 2 * c.HB, fp32=True)
                    n_mix_blocks = len(blocks)
                    mk_F('mo', (moT, 'moT'), AF.Sigmoid, 1.0)
                    mk_F('ga', (gT, 'gT'), AF.Sigmoid, 1.0, dst_row0=0)
                    mk_F('gb', (gT, 'gT'), AF.Sigmoid, 1.0, dst_row0=D)
                    mk_F('gc', (gT, 'gT'), AF.Sigmoid, 1.0, dst_row0=2 * D)

                    cc_list = [(gifs[:, :], gifg[:, :])]
                    for k, rk in enumerate(fm_rows):
                        cc_list.append((FMs[k * CHF:k * CHF + rk, :], FMg_l[k][:, :]))
                    for k in range(TH // CHT):
                        cc_list.append((TMs[k * CHT:(k + 1) * CHT, :], TMg_l[k][:, :]))
                    cc_sems = [es.enter_context(nc.semaphore(f"cc_{l}_{i}")) for i in range(len(cc_list))]

                    def issue_cc():
                        deps = {}
                        for n in ['qaT', 'kaT', 'va', 'mqT', 'mkT', 'mv', 'cqT', 'ckT', 'cv', 'gif']:
                            _merge(deps, B_scr[n].w)
                        kb._waits('pool', deps)
                        for i, (src_, dst_) in enumerate(cc_list):
                            nc.gpsimd.collective_compute("AllGather", ALU.bypass, replica_groups=PAIRS, ins=[src_], outs=[dst_]).then_inc(cc_sems[i], 1)
                    blocks[n_mix_blocks]['before_mm'] = issue_cc
                    gemm_half(actA, B_A, DC, blocks, wbufs, Bw, wctr)
                kb.barrier()
            for e_name in kb.eng:
                for i in range(len(cc_list)):
                    kb.eng[e_name].wait_ge(cc_sems[i], 1)
            NGH = 2 * c.HB + c.HC
            NFH = c.HB + c.HC
            with ExitStack() as es_l:
                es2 = es_l
                Gs = sb("B_Gs", [128, NB, NGH], F32)
                bcat = sb("B_bcat", [128, NGH], F32)
                nlf = sb("B_nlf", [128, NB, NFH], F32)
                negB = sb("B_negB", [128, NB, NFH], F32)
                a_m = sb("B_am", [128, NB, c.HB], F32)
                B_g = Buf()
                kb.dma('sp', Gs[:], gifg.rearrange("(j p) g -> p j g", p=128), reads=[B_scr['gif']], writes=[B_g])

                def bc_row(src1d, n):
                    return bass.AP(src1d.tensor, src1d.offset, [[0, 128], [1, n]])
                kb.dma('sp', bcat[:, 0:c.HB], bc_row(b_i[l], c.HB), awrites=[B_g])
                kb.dma('sp', bcat[:, c.HB:2 * c.HB], bc_row(b_f[l], c.HB), awrites=[B_g])
                kb.dma('sp', bcat[:, 2 * c.HB:NGH], bc_row(b_cf[l], c.HC), awrites=[B_g])
                B_g2 = Buf()

                def addb(e):
                    ins = None
                    for j in range(NB):
                        ins = e.tensor_tensor(out=Gs[:, j, :], in0=Gs[:, j, :], in1=bcat[:, :], op=ALU.add)
                    return ins
                kb.op('dve', addb, reads=[B_g], writes=[B_g2])
                kb.op('act', lambda e: e.activation(out=nlf[:], in_=Gs[:, :, c.HB:NGH], func=AF.Exp, scale=-1.0), reads=[B_g2], writes=[B_g])
                B_nlf = Buf()
                kb.op('act', lambda e: e.activation(out=nlf[:], in_=nlf[:], func=AF.Ln, bias=1.0, scale=1.0), reads=[B_g], writes=[B_nlf])
                B_negB = Buf()
                for j in range(NB):
                    ps, pb = psum[j % 4], PB[j % 4]

                    def mm(e, j=j, ps=ps):
                        ins = e.matmul(ps[:, :NFH], lhsT=tri_f[:], rhs=nlf[:, j, :], start=True, stop=(j == 0))
                        for jj in range(j):
                            ins = e.matmul(ps[:, :NFH], lhsT=ones_f[:], rhs=nlf[:, jj, :], start=False, stop=(jj == j - 1))
                        return ins
                    kb.op('pe', mm, reads=[B_nlf, B_const], writes=[pb])
                    kb.op('dve', lambda e, j=j, ps=ps: e.tensor_copy(out=negB[:, j, :], in_=ps[:, :NFH]), reads=[pb], awrites=[B_negB])
                B_am = Buf()
                kb.op('dve', lambda e: e.tensor_tensor(out=a_m[:], in0=Gs[:, :, 0:c.HB], in1=negB[:, :, 0:c.HB], op=ALU.add),
                      reads=[B_g2, B_negB], writes=[B_am])

                brow = [sb(f"B_brow{i}", [128, T], F32) for i in range(2)]
                B_brow = [Buf(), Buf()]
                bbt = Ring([sb(f"B_bb{i}", [128, 128], F32) for i in range(3)])

                def make_brow(hd, slot):
                    for j4 in range(NB // 4):
                        ps, pb = psum[0], PB[0]
                        for k in range(4):
                            j = j4 * 4 + k
                            bb, bbb = bbt.next()
                            kb.op('dve', lambda e, j=j, bb=bb: e.tensor_scalar_mul(out=bb[:], in0=ones_f[:], scalar1=negB[:, j, hd:hd + 1]), reads=[B_negB, B_const], writes=[bbb])
                            kb.op('pe', lambda e, k=k, bb=bb, ps=ps: e.matmul(ps[:, k * 128:(k + 1) * 128], lhsT=bb[:], rhs=ident[:], start=True, stop=True),
                                  reads=[bbb, B_const], **({'writes': [pb]} if k == 0 else {'awrites': [pb]}))
                        kb.op('act', lambda e, j4=j4, ps=ps: e.activation(out=brow[slot][:, j4 * 512:(j4 + 1) * 512], in_=ps[:, :], func=AF.Copy, scale=-1.0),
                              reads=[pb], **({'writes': [B_brow[slot]]} if j4 == 0 else {'awrites': [B_brow[slot]]}))

                qh = [sb(f"B_qh{i}", [128, T], BF16) for i in range(2)]
                kh = [sb(f"B_kh{i}", [128, T], BF16) for i in range(2)]
                vh = [sb(f"B_vh{i}", [128, NB, 128], BF16) for i in range(2)]
                B_qkv = [Buf(), Buf()]
                pTr = Ring([sb(f"B_pT{i}", [128, 512], BF16) for i in range(5)])
                wTr = Ring([sb(f"B_wT{i}", [128, 512], F32) for i in range(4)])
                crow_b = [sb(f"B_crow{i}", [1, T], BF16) for i in range(2)]
                B_crow = [Buf(), Buf()]
                rlr = Ring([sb(f"B_rl{i}", [128, 512], F32) for i in range(2)])
                yst = Ring([sb(f"B_yst{i}", [128, 512], BF16) for i in range(3)])
                NTG4 = T // 512
                heads = [('mlstm', h) for h in range(c.HB)] + [('fox', h) for h in range(c.HC)]

                def head_params(kind, h):
                    if kind == 'fox':
                        return dict(hd=c.HB + h, qrow=o_cq + h * 128, krow=o_ck + h * 128, vc0=128 + c.B_V + h * 128, KP=128, p0=0,
                                    yrow=c.A_Q + c.B_V + h * 128)
                    return dict(hd=h, qrow=o_mq + (h // 2) * 128, krow=o_mk + (h // 2) * 128, vc0=128 + h * 128, KP=64, p0=(h % 2) * 64,
                                yrow=c.A_Q + h * 128)

                def load_head(idx):
                    kind, h = heads[idx]
                    slot = idx % 2
                    hp = head_params(kind, h)
                    first = True
                    for dst_t, row0 in ((qh[slot], hp['qrow']), (kh[slot], hp['krow'])):
                        for r in range(2):
                            kb.dma('sp', dst_t[:, r * TH:(r + 1) * TH], fm_g(r, row0), **({'writes': [B_qkv[slot]]} if first else {'awrites': [B_qkv[slot]]}))
                            first = False
                    for jb in range(NB):
                        kb.dma('sp', vh[slot][:, jb, :], tm_g(jb)[:, hp['vc0']:hp['vc0'] + 128], awrites=[B_qkv[slot]])
                    make_brow(hp['hd'], slot)
                    if kind == 'fox':
                        kb.op('act', lambda e: e.activation(out=crow_b[slot][0:1, :], in_=brow[slot][0:1, :], func=AF.Copy), reads=[B_brow[slot]], writes=[B_crow[slot]])

                def compute_head(idx):
                    kind, h = heads[idx]
                    slot = idx % 2
                    hp = head_params(kind, h)
                    hd, KP, p0 = hp['hd'], hp['KP'], hp['p0']
                    q_, k_, v_, br = qh[slot], kh[slot], vh[slot], brow[slot]
                    iters = [(tg, sbk) for tg in range(NTG4) for sbk in range(4 * tg + 4)]
                    state = {}

                    def stageA(ii):
                        tg, sbk = iters[ii]
                        d = sbk - 4 * tg
                        c0 = max(d, 0) * 128
                        psS, pbS = psum[1 + ii % 3], PB[1 + ii % 3]
                        tcol = slice(tg * 512 + c0, (tg + 1) * 512)
                        pT, bpT = pTr.next()
                        if kind == 'fox':
                            def mm(e):
                                e.matmul(psS[:, c0:], lhsT=k_[:, sbk * 128:(sbk + 1) * 128], rhs=q_[:, tcol], start=True, stop=False)
                                return e.matmul(psS[:, c0:], lhsT=ones_b[0:1, :], rhs=crow_b[slot][0:1, tcol], start=False, stop=True)
                            kb.op('pe', mm, reads=[B_qkv[slot], B_crow[slot], B_const], writes=[pbS])
                            kb.op('act', lambda e: e.activation(out=pT[:, c0:], in_=psS[:, c0:], func=AF.Exp, bias=negB[:, sbk, hd:hd + 1], scale=1.0),
                                  reads=[pbS, B_negB], writes=[bpT])
                        else:
                            kb.op('pe', lambda e: e.matmul(psS[:, c0:], lhsT=k_[p0:p0 + KP, sbk * 128:(sbk + 1) * 128], rhs=q_[p0:p0 + KP, tcol], start=True, stop=True),
                                  reads=[B_qkv[slot]], writes=[pbS])
                            wT, bwT = wTr.next()
                            kb.op('act', lambda e: e.activation(out=wT[:, c0:], in_=br[:, tcol], func=AF.Exp, bias=a_m[:, sbk, hd:hd + 1], scale=1.0),
                                  reads=[B_brow[slot], B_am], writes=[bwT])
                            kb.op('dve', lambda e: e.tensor_tensor(out=pT[:, c0:], in0=psS[:, c0:], in1=wT[:, c0:], op=ALU.mult),
                                  reads=[pbS, bwT], writes=[bpT])
                        if d >= 0:
                            if c0 > 0:
                                kb.op('dve', lambda e: e.memset(pT[:, 0:c0], 0.0), awrites=[bpT])
                            kb.op('dve', lambda e: e.tensor_tensor(out=pT[:, c0:c0 + 128], in0=pT[:, c0:c0 + 128], in1=tri_b[:], op=ALU.mult),
                                  reads=[bpT, B_const], writes=[bpT])
                        state[ii] = (pT, bpT)

                    def stageB(ii):
                        tg, sbk = iters[ii]
                        nsb = 4 * tg + 4
                        psO, pbO = psum[4 + (tg % 2)], PB[4 + (tg % 2)]
                        psL, pbL = psum[6 + (tg % 2)], PB[6 + (tg % 2)]
                        pT, bpT = state.pop(ii)
                        first, last = (sbk == 0), (sbk == nsb - 1)
                        kb.op('pe', lambda e: e.matmul(psO[:, :], lhsT=v_[:, sbk, :], rhs=pT[:, :], start=first, stop=last),
                              reads=[bpT, B_qkv[slot]], **({'writes': [pbO]} if first else {'awrites': [pbO]}))
                        kb.op('pe', lambda e: e.matmul(psL[:, :], lhsT=ones_b[:], rhs=pT[:, :], start=first, stop=last),
                              reads=[bpT, B_const], **({'writes': [pbL]} if first else {'awrites': [pbL]}))
                        if last:
                            yst_t, byst = yst.next()
                            rl, B_rl = rlr.next()
                            if kind == 'fox':
                                kb.op('dve', lambda e: e.reciprocal(out=rl[:], in_=psL[:, :]), reads=[pbL], writes=[B_rl])
                            else:
                                kb.op('act', lambda e: e.activation(out=rl[:], in_=psL[:, :], func=AF.Abs), reads=[pbL], writes=[B_rl])
                                kb.op('dve', lambda e: e.tensor_scalar_max(out=rl[:], in0=rl[:], scalar1=1.0), reads=[B_rl], writes=[B_rl])
                                kb.op('dve', lambda e: e.reciprocal(out=rl[:], in_=rl[:]), reads=[B_rl], writes=[B_rl])
                            kb.op('dve', lambda e: e.tensor_tensor(out=yst_t[:], in0=psO[:, :], in1=rl[:], op=ALU.mult), reads=[pbO, B_rl], writes=[byst])
                            kb.dma('sp', yT[hp['yrow']:hp['yrow'] + 128, tg * 512:(tg + 1) * 512], yst_t[:], reads=[byst], awrites=[B_scr['yT']])

                    stageA(0)
                    stageA(1)
                    for ii in range(len(iters)):
                        if ii + 2 < len(iters):
                            stageA(ii + 2)
                        stageB(ii)

                NJ = c.HA // 2
                qT_all = sb("S_q", [128, NJ, T], BF16)
                kT2 = sb("S_k", [128, 2, T], BF16)
                vraw = sb("S_vraw", [128, NB, 128], BF16)
                Vp = [[sb(f"S_V{g}{e}", [128, NB, 128], BF16) for e in range(2)] for g in range(2)]
                ones_lo = sb("S_olo", [128, 128], BF16)
                ones_hi = sb("S_ohi", [128, 128], BF16)
                Etab = sb("S_E", [128, c.HA, 256], F32)
                esink = sb("S_es", [128, NJ], F32)
                B_s = Buf()
                first_ = True
                for r in range(2):
                    for j in range(NJ):
                        kb.dma('sp', qT_all[:, j, r * TH:(r + 1) * TH], fm_g(r, o_qa + j * 128), **({'writes': [B_s]} if first_ else {'awrites': [B_s]}))
                        first_ = False
                    for j in range(2):
                        kb.dma('sp', kT2[:, j, r * TH:(r + 1) * TH], fm_g(r, o_ka + j * 128), awrites=[B_s])
                for jb in range(NB):
                    kb.dma('sp', vraw[:, jb, :], tm_g(jb)[:, 0:128], awrites=[B_s])
                kb.dma('sp', Etab[:], c_alibi.rearrange("p (h c) -> p h c", c=256), awrites=[B_s])
                sk = sinks[l]
                for e_ in range(2):
                    kb.dma('sp', esink[e_ * 64:(e_ + 1) * 64, :], bass.AP(sk.tensor, sk.offset + e_, [[0, 64], [2, NJ]]), awrites=[B_s], slow=True)
                B_s2 = Buf()

                kb.op('dve', lambda e: e.memset(ones_lo[:], 0.0), writes=[B_s2])
                kb.op('dve', lambda e: e.memset(ones_hi[:], 0.0), reads=[B_s2], writes=[B_s2])
                kb.op('dve', lambda e: e.memset(ones_lo[:, 0:64], 1.0), reads=[B_s2], writes=[B_s2])
                kb.op('dve', lambda e: e.memset(ones_hi[:, 64:128], 1.0), reads=[B_s2], writes=[B_s2])
                for g in range(2):
                    for e_ in range(2):
                        kb.op('dve', lambda e, g=g, e_=e_: e.memset(Vp[g][e_][:], 0.0), reads=[B_s2], writes=[B_s2])

                def prep2(e):
                    ins = None
                    for g in range(2):
                        e.tensor_copy(out=Vp[g][0][:, :, 0:64], in_=vraw[:, :, g * 64:(g + 1) * 64])
                        ins = e.tensor_copy(out=Vp[g][1][:, :, 64:128], in_=vraw[:, :, g * 64:(g + 1) * 64])
                    return ins
                kb.op('dve', prep2, reads=[B_s, B_s2], writes=[B_s2])
                kb.op('act', lambda e: e.activation(out=esink[:], in_=esink[:], func=AF.Exp), reads=[B_s], writes=[B_s])
                load_head(0)
                for idx in range(len(heads)):
                    if idx + 1 < len(heads):
                        load_head(idx + 1)
                    compute_head(idx)
                pex = Ring([sb(f"S_pex{i}", [128, 256], F32) for i in range(4)])
                pTs = Ring([sb(f"S_pT{i}", [128, 256], BF16) for i in range(6)])
                lsr = Ring([sb(f"S_ls{i}", [128, 128], F32) for i in range(2)])
                ysts = Ring([sb(f"S_yst{i}", [128, 128], BF16) for i in range(3)])
                sw_its = [(qb, j) for qb in range(NB) for j in range(NJ)]
                sw_state = {}

                def swA(it):
                    qb, j = sw_its[it]
                    qs = slice(qb * 128, (qb + 1) * 128)
                    cl = 128 if qb == 0 else 0
                    lst = []
                    for e_ in range(2):
                        h = 2 * j + e_
                        g = h // c.GRP
                        kc = 0 if g == e_ else 1
                        pp = slice(e_ * 64, (e_ + 1) * 64)
                        psS, pbS = psum[(2 * it + e_) % 4], PB[(2 * it + e_) % 4]

                        def mm(e, psS=psS, pp=pp, kc=kc):
                            ins = e.matmul(psS[:, 128:256], lhsT=kT2[pp, kc, qs], rhs=qT_all[pp, j, qs], start=True, stop=True)
                            if qb > 0:
                                ins = e.matmul(psS[:, 0:128], lhsT=kT2[pp, kc, (qb - 1) * 128:qb * 128], rhs=qT_all[pp, j, qs], start=True, stop=True)
                            return ins
                        kb.op('pe', mm, reads=[B_s], writes=[pbS])
                        px, bpx = pex.next()
                        kb.op('act', lambda e, px=px, psS=psS: e.activation(out=px[:, cl:], in_=psS[:, cl:256], func=AF.Exp), reads=[pbS], writes=[bpx])
                        pT, bpT = pTs.next()
                        kb.op('dve', lambda e, px=px, pT=pT, h=h: e.tensor_tensor(out=pT[:, cl:], in0=px[:, cl:], in1=Etab[:, h, cl:], op=ALU.mult),
                              reads=[bpx, B_s], writes=[bpT])
                        lst.append((pT, bpT, g, e_))
                    sw_state[it] = lst

                def swB(it):
                    qb, j = sw_its[it]
                    qs = slice(qb * 128, (qb + 1) * 128)
                    psO, pbO = psum[4 + (it % 2)], PB[4 + (it % 2)]
                    psL, pbL = psum[6 + (it % 2)], PB[6 + (it % 2)]
                    nmm = 0
                    tot = 2 * (1 if qb == 0 else 2)
                    for (pT, bpT, g, e_) in sw_state.pop(it):
                        for kbk in ((1,) if qb == 0 else (0, 1)):
                            first, last = (nmm == 0), (nmm == tot - 1)
                            nmm += 1
                            vb = qb - 1 + kbk
                            cs = slice(kbk * 128, (kbk + 1) * 128)
                            kb.op('pe', lambda e, pT=pT, g=g, e_=e_, vb=vb, cs=cs, first=first, last=last:
                                  e.matmul(psO[:, 0:128], lhsT=Vp[g][e_][:, vb, :], rhs=pT[:, cs], start=first, stop=last),
                                  reads=[bpT, B_s2], **({'writes': [pbO]} if first else {'awrites': [pbO]}))
                            kb.op('pe', lambda e, pT=pT, e_=e_, cs=cs, first=first, last=last:
                                  e.matmul(psL[:, 0:128], lhsT=(ones_lo if e_ == 0 else ones_hi)[:], rhs=pT[:, cs], start=first, stop=last),
                                  reads=[bpT, B_s2], **({'writes': [pbL]} if first else {'awrites': [pbL]}))
                    ls, B_ls = lsr.next()
                    kb.op('dve', lambda e: e.tensor_scalar_add(out=ls[:], in0=psL[:, 0:128], scalar1=esink[:, j:j + 1]), reads=[pbL, B_s], writes=[B_ls])
                    kb.op('dve', lambda e: e.reciprocal(out=ls[:], in_=ls[:]), reads=[B_ls], writes=[B_ls])
                    ys, bys = ysts.next()
                    kb.op('dve', lambda e: e.tensor_tensor(out=ys[:], in0=psO[:, 0:128], in1=ls[:], op=ALU.mult), reads=[pbO, B_ls], writes=[bys])
                    kb.dma('sp', yT[j * 128:(j + 1) * 128, qs], ys[:], reads=[bys], awrites=[B_scr['yT']])

                swA(0)
                for it in range(len(sw_its)):
                    if it + 1 < len(sw_its):
                        swA(it + 1)
                    swB(it)
                kb.barrier()


            x_dst = y_out if l == L - 1 else x2_scr
            B_xdst = Buf()
            for hf in range(1):
                t0 = 0
                with ExitStack() as es_o:
                    es2 = es_o
                    actA = sb("C_act", [128, DC, TH], BF16)
                    B_A = Buf()
                    with ExitStack() as es_l:
                        es2 = es_l
                        KU = [c.A_Q // 128, c.B_V // 128, c.C_W // 128]
                        yrow = [0, c.A_Q, c.A_Q + c.B_V]
                        ybuf = [sb(f"C_y{i}", [128, KU[i], TH], BF16) for i in range(3)]
                        B_y = Buf()
                        ytmp = sb("C_ytmp", [128, max(KU), TH], BF16)
                        B_yt = Buf()
                        for br_ in range(3):
                            yv = yT[yrow[br_]:yrow[br_] + KU[br_] * 128, :].rearrange("(k p) t -> p k t", p=128)
                            yb_, yt_ = ybuf[br_], ytmp[:, :KU[br_], :]
                            kb.dma('sp', yb_[:], yv[:, :, 0:TH], reads=[B_scr['yT']], **({'writes': [B_y]} if br_ == 0 else {'awrites': [B_y]}))
                            kb.dma('sp', yt_, yv[:, :, TH:2 * TH], reads=[B_scr['yT']], writes=[B_yt])
                            kb.op('dve', lambda e, yb_=yb_: e.tensor_scalar_mul(out=yb_[:], in0=yb_[:], scalar1=fl[:, 0:1]), reads=[B_y, B_const], writes=[B_y])
                            kb.op('dve', lambda e, yb_=yb_, yt_=yt_: e.scalar_tensor_tensor(out=yb_[:], in0=yt_, scalar=fl[:, 1:2], in1=yb_[:], op0=ALU.mult, op1=ALU.add),
                                  reads=[B_y, B_yt, B_const], writes=[B_y])
                            if br_ == 1:
                                kb.dma('sp', yt_, moT.rearrange("(k p) t -> p k t", p=128), reads=[B_scr['moT']], writes=[B_yt])
                                kb.op('dve', lambda e, yb_=yb_, yt_=yt_: e.tensor_tensor(out=yb_[:], in0=yb_[:], in1=yt_, op=ALU.mult), reads=[B_y, B_yt], writes=[B_y])
                        wub = [sb(f"C_wu{i}", [128, max(KU), 512], BF16) for i in range(3)]
                        Bwu = [Buf() for _ in range(3)]
                        gtr = Ring([sb(f"C_gt{i}", [128, 512], BF16) for i in range(4)])
                        acc = Ring([sb(f"C_acc{i}", [128, 512], F32) for i in range(2)])
                        tmpr = Ring([sb(f"C_tmp{i}", [128, 512], F32) for i in range(2)])
                        wi = 0
                        bank = 0
                        for cb in range(D // 512):
                            wsel = []
                            for br_ in range(3):
                                wbuf, bw = wub[wi % 3], Bwu[wi % 3]
                                wi += 1
                                kb.dma('pool', wbuf[:, :KU[br_], :], wview(w_up[br_][l], 0, KU[br_], cb * 512, 512), writes=[bw])
                                wsel.append((wbuf, bw))
                            for cc in range(4):
                                for tg in range(NTG):
                                    ac, bac = acc.next()
                                    chunk = cb * 4 + cc
                                    for br_ in range(3):
                                        ps, pb = psum[bank % 8], PB[bank % 8]
                                        bank += 1
                                        wbuf, bw = wsel[br_]

                                        def mm(e, ps=ps, wbuf=wbuf, br_=br_, cc=cc, tg=tg):
                                            ins = None
                                            for k in range(KU[br_]):
                                                ins = e.matmul(ps[:, :], lhsT=wbuf[:, k, cc * 128:(cc + 1) * 128], rhs=ybuf[br_][:, k, tg * 512:(tg + 1) * 512],
                                                               start=(k == 0), stop=(k == KU[br_] - 1))
                                            return ins
                                        kb.op('pe', mm, reads=[bw, B_y], writes=[pb])
                                        gt, bgt = gtr.next()
                                        r0 = br_ * D + chunk * 128
                                        kb.dma('sp', gt[:], gT[r0:r0 + 128, t0 + tg * 512:t0 + (tg + 1) * 512], reads=[B_scr['gT']], writes=[bgt])
                                        if br_ == 0:
                                            kb.op('dve', lambda e, ac=ac, ps=ps, gt=gt: e.tensor_tensor(out=ac[:], in0=ps[:, :], in1=gt[:], op=ALU.mult),
                                                  reads=[pb, bgt], writes=[bac])
                                        else:
                                            tm, btm = tmpr.next()
                                            kb.op('dve', lambda e, tm=tm, ps=ps, gt=gt: e.tensor_tensor(out=tm[:], in0=ps[:, :], in1=gt[:], op=ALU.mult),
                                                  reads=[pb, bgt], writes=[btm])
                                            if br_ == 1:
                                                kb.op('dve', lambda e, ac=ac, tm=tm: e.tensor_tensor(out=ac[:], in0=ac[:], in1=tm[:], op=ALU.add),
                                                      reads=[btm, bac], writes=[bac])
                                            else:
                                                kb.op('dve', lambda e, ac=ac, tm=tm, chunk=chunk, tg=tg: e.tensor_tensor(out=actA[:, chunk, tg * 512:(tg + 1) * 512], in0=ac[:], in1=tm[:], op=ALU.add),
                                                      reads=[btm, bac], awrites=[B_A])
                        kb.barrier()

                    def gemm_resid(W2d, KTOT, act_loader, res_src, B_res, tagp, B_wsrc=None):
                        with ExitStack() as es_l:
                            nonlocal es2
                            es2 = es_l
                            KP = 16 if KTOT > DC else DC
                            npieces = KTOT // KP
                            wb_ = [sb(f"{tagp}_w{i}", [128, KP, 512], BF16) for i in range(2)]
                            Bw_ = [Buf(), Buf()]
                            xr = Ring([sb(f"{tagp}_xr{i}", [128, 512], F32) for i in range(3)])
                            ro = Ring([sb(f"{tagp}_ro{i}", [128, 512], F32) for i in range(3)])
                            wi = 0
                            for cb in range(D // 512):
                                for kp in range(npieces):
                                    wbuf, bw = wb_[wi % 2], Bw_[wi % 2]
                                    wi += 1
                                    kb.dma('pool', wbuf[:], wview(W2d, kp * KP, KP, cb * 512, 512), reads=([B_wsrc] if B_wsrc is not None else []), writes=[bw])
                                    at, bat, koff = act_loader(kp, KP)
                                    for tt in range(NTT):
                                        ps, pb = psum[tt], PB[tt]

                                        def mm(e, ps=ps, wbuf=wbuf, at=at, koff=koff, tt=tt, kp=kp):
                                            ins = None
                                            for k in range(KP):
                                                ins = e.matmul(ps[:, :], lhsT=at[:, koff + k, tt * 128:(tt + 1) * 128], rhs=wbuf[:, k, :],
                                                               start=(kp == 0 and k == 0), stop=(kp == npieces - 1 and k == KP - 1))
                                            return ins
                                        kb.op('pe', mm, reads=[bw, bat], **({'writes': [pb]} if kp == 0 else {'awrites': [pb]}))
                                        if kp == npieces - 1:
                                            xt_, bx_ = xr.next()
                                            kb.dma('sp', xt_[:], res_src[t0 + tt * 128:t0 + (tt + 1) * 128, cb * 512:(cb + 1) * 512], reads=[B_res], writes=[bx_])
                                            rt_, br__ = ro.next()
                                            kb.op('dve', lambda e, rt_=rt_, xt_=xt_, ps=ps: e.scalar_tensor_tensor(out=rt_[:], in0=xt_[:], scalar=c.alpha, in1=ps[:, :],
                                                                                                                 op0=ALU.mult, op1=ALU.add),
                                                  reads=[pb, bx_], writes=[br__])
                                            kb.dma('sp', r_scr[t0 + tt * 128:t0 + (tt + 1) * 128, cb * 512:(cb + 1) * 512], rt_[:], reads=[br__], awrites=[B_scr['r']])
                            kb.barrier()

                    def ln_pass(g_ap, b_ap, dst, B_dst, tagp, also_xT_scr):
                        with ExitStack() as es_l:
                            nonlocal es2
                            es2 = es_l
                            gam = sb(f"{tagp}_g", [128, D], F32)
                            bet = sb(f"{tagp}_b", [128, D], F32)
                            B_gb = Buf()
                            kb.dma('sp', gam[:], bass.AP(g_ap.tensor, g_ap.offset, [[0, 128], [1, D]]), writes=[B_gb])
                            kb.dma('sp', bet[:], bass.AP(b_ap.tensor, b_ap.offset, [[0, 128], [1, D]]), awrites=[B_gb])
                            NR = 3
                            rt = [sb(f"{tagp}_rt{i}", [128, D], F32) for i in range(NR)]
                            Brt = [Buf() for _ in range(NR)]
                            junk = sb(f"{tagp}_junk", [128, D], BF16)
                            B_junk = Buf()
                            stat = [sb(f"{tagp}_st{i}", [128, 8], F32) for i in range(NR)]
                            Bst = [Buf() for _ in range(NR)]
                            rb = [sb(f"{tagp}_rb{i}", [128, D], BF16) for i in range(NR)]
                            Brb = [Buf() for _ in range(NR)]

                            def lnA(tt):
                                i = tt % NR
                                r_, s_, bs_ = rt[i], stat[i], Bst[i]
                                rows = slice(t0 + tt * 128, t0 + (tt + 1) * 128)
                                kb.dma('sp', r_[:], r_scr[rows, :], reads=[B_scr['r']], writes=[Brt[i]])
                                kb.op('dve', lambda e: e.memset(s_[:], 0.0), writes=[bs_])
                                kb.op('act', lambda e: e.activation(out=junk[:], in_=r_[:], func=AF.Copy, accum_out=s_[:, 0:1]),
                                      reads=[Brt[i]], writes=[B_junk], awrites=[bs_])
                                kb.op('act', lambda e: e.activation(out=junk[:], in_=r_[:], func=AF.Square, accum_out=s_[:, 1:2]),
                                      reads=[Brt[i]], writes=[B_junk], awrites=[bs_])
                                kb.op('dve', lambda e: e.tensor_scalar_mul(out=s_[:, 2:3], in0=s_[:, 0:1], scalar1=1.0 / D), reads=[bs_], writes=[bs_])
                                kb.op('dve', lambda e: e.tensor_tensor(out=s_[:, 3:4], in0=s_[:, 2:3], in1=s_[:, 2:3], op=ALU.mult), reads=[bs_], writes=[bs_])
                                kb.op('dve', lambda e: e.scalar_tensor_tensor(out=s_[:, 4:5], in0=s_[:, 1:2], scalar=1.0 / D, in1=s_[:, 3:4], op0=ALU.mult, op1=ALU.subtract),
                                      reads=[bs_], writes=[bs_])
                                kb.op('act', lambda e: e.activation(out=s_[:, 6:7], in_=s_[:, 4:5], func=AF.Sqrt, bias=1e-5, scale=1.0), reads=[bs_], writes=[bs_])
                                kb.op('dve', lambda e: e.reciprocal(out=s_[:, 7:8], in_=s_[:, 6:7]), reads=[bs_], writes=[bs_])
                                kb.op('dve', lambda e: e.scalar_tensor_tensor(out=s_[:, 5:6], in0=s_[:, 2:3], scalar=-1.0, in1=s_[:, 7:8], op0=ALU.mult, op1=ALU.mult),
                                      reads=[bs_], writes=[bs_])
                                kb.op('act', lambda e: e.activation(out=r_[:], in_=r_[:], func=AF.Identity, scale=s_[:, 7:8], bias=s_[:, 5:6]),
                                      reads=[bs_, Brt[i]], writes=[Brt[i]])

                            def lnB(tt):
                                i = tt % NR
                                r_ = rt[i]
                                rows = slice(t0 + tt * 128, t0 + (tt + 1) * 128)
                                kb.op('dve', lambda e: e.tensor_tensor(out=r_[:], in0=r_[:], in1=gam[:], op=ALU.mult), reads=[Brt[i], B_gb], writes=[Brt[i]])
                                kb.op('dve', lambda e: e.tensor_tensor(out=rb[i][:], in0=r_[:], in1=bet[:], op=ALU.add), reads=[Brt[i], B_gb], writes=[Brb[i]])
                                kb.op('pool', lambda e: e.tensor_tensor(out=r_[:], in0=r_[:], in1=bet[:], op=ALU.add), reads=[Brt[i], B_gb], writes=[Brt[i]])
                                kb.dma('sp', dst[rows, :], r_[:], reads=[Brt[i]], awrites=[B_dst])
                                transpose_tile(rb[i], Brb[i], lambda cq: actA[:, cq * 4:(cq + 1) * 4, tt * 128:(tt + 1) * 128], B_A, 'awrites')

                            lnA(0)
                            for tt in range(NTT):
                                if tt + 1 < NTT:
                                    lnA(tt + 1)
                                lnB(tt)
                            kb.barrier()
                            if also_xT_scr:
                                kb.dma('sp', xT_v[:, :, t0:t0 + TH], actA[:], reads=[B_A], awrites=[B_xT])
                                kb.barrier()

                    gemm_resid(w_o[l], DC, lambda kp, KP: (actA, B_A, 0), x_res, B_xres, "Wo")
                    B_A = Buf()
                    ln_pass(ln1_g[l], ln1_b[l], x1_scr, B_scr['x1'], "L1", False)
                    with ExitStack() as es_l:
                        es2 = es_l
                        wbufs = [sb(f"F1_w{i}", [128, DC, 512], BF16) for i in range(2)]
                        Bw = [Buf(), Buf()]
                        wctr = [0]
                        rl_ = Ring([sb(f"F1_r{i}", [128, 512], F32) for i in range(3)])
                        hs = Ring([sb(f"F1_h{i}", [128, 512], BF16) for i in range(4)])
                        blocks = []
                        for c0 in range(0, DFF, 512):
                            def wload(wb, c0=c0):
                                return [(wb[:, :, :], wview(w_ff1[l], 0, DC, c0, 512))]

                            def epi(cc, tg, ps, pb, c0=c0):
                                rr, brr = rl_.next()
                                kb.op('act', lambda e: e.activation(out=rr[:], in_=ps[:, :], func=AF.Relu), reads=[pb], writes=[brr])
                                hh, bhh = hs.next()
                                kb.op('dve', lambda e: e.tensor_tensor(out=hh[:], in0=rr[:], in1=rr[:], op=ALU.mult), reads=[brr], writes=[bhh])
                                r0 = c0 + cc * 128
                                kb.dma('sp', hidT[r0:r0 + 128, tg * 512:(tg + 1) * 512], hh[:], reads=[bhh], awrites=[B_scr['hid']])
                            blocks.append(dict(wload=wload, ncols=512, orient='F', epi=epi))
                        gemm_half(actA, B_A, DC, blocks, wbufs, Bw, wctr)
                        kb.barrier()
                    with ExitStack() as es_l:
                        es2 = es_l
                        hb = [sb(f"F2_h{i}", [128, 16, TH], BF16) for i in range(2)]
                        Bhb = [Buf(), Buf()]
                        hctr = [0]
                        hid_v = hidT.rearrange("(k p) t -> p k t", p=128)

                        def hid_loader(kp, KP):
                            i = hctr[0] % 2
                            hctr[0] += 1
                            kb.dma('act', hb[i][:], hid_v[:, kp * KP:(kp + 1) * KP, :], reads=[B_scr['hid']], writes=[Bhb[i]])
                            return hb[i], Bhb[i], 0
                        gemm_resid(w_ff2[l], DFF // 128, hid_loader, x1_scr, B_scr['x1'], "F2")
                    B_A = Buf()
                    B_scr['hid'] = Buf()
                    ln_pass(ln2_g[l], ln2_b[l], x_dst, B_xdst, "L2", l < L - 1)
            x_res = x2_scr
            B_xres = B_xdst
        kb.final_wait()
    return nc


def _consts(cfg):
    ident = np.eye(128, dtype=np.float32)
    tri = np.triu(np.ones((128, 128), dtype=np.float32))
    slopes = 2.0 ** (-8.0 * np.arange(1, cfg.HA + 1, dtype=np.float64) / cfg.HA)
    k = np.arange(128)[:, None].astype(np.float64)
    q = np.arange(128)[None, :].astype(np.float64)
    E = np.zeros((128, cfg.HA, 256), dtype=np.float32)
    for h in range(cfg.HA):
        dprev = q + 128 - k
        E[:, h, 0:128] = np.where(dprev < 128, np.exp(-slopes[h] * dprev), 0.0)
        dcur = q - k
        E[:, h, 128:256] = np.where(dcur >= 0, np.exp(-slopes[h] * dcur), 0.0)
    return ident, tri, E.reshape(128, cfg.HA * 256)


_NAMES = ['b_mlstm_i', 'b_mlstm_f', 'b_fox_f', 'attn_sinks', 'w_up_swa', 'w_up_mlstm', 'w_up_fox', 'w_o',
          'ln1_g', 'ln1_b', 'ln2_g', 'ln2_b']
_SPLIT = ['w_in', 'w_ff1', 'w_ff2']


def run(cfg, inputs, debug=False):
    nc = build_program(cfg, debug=debug)
    ident, tri, E = _consts(cfg)
    x = np.asarray(inputs['x'], dtype=np.float32)
    B = x.shape[0]
    NCr = cfg.NCORES
    assert 2 * B == NCr
    TH = cfg.TH
    shared = {n: np.ascontiguousarray(np.asarray(inputs[n], dtype=np.float32)) for n in _NAMES}
    for n in _SPLIT:
        a = np.asarray(inputs[n], dtype=np.float32)
        for i in range(cfg.L):
            shared[f"{n}_{i}"] = np.ascontiguousarray(a[i])
    in_maps = []
    for cidx in range(NCr):
        b, p = cidx // 2, cidx % 2
        m = dict(shared)
        m['x'] = np.ascontiguousarray(x[b, p * TH:(p + 1) * TH])
        m['flags'] = np.array([[1.0 - p, float(p)]], dtype=np.float32)
        m['c_ident'] = ident
        m['c_tri'] = tri
        m['c_alibi'] = E
        in_maps.append(m)
    res = run_bass_kernel_spmd(nc, in_maps, core_ids=list(range(NCr)))
    if debug:
        return res.results
    out = np.empty((B, cfg.T, cfg.D), dtype=np.float32)
    for cidx in range(NCr):
        b, p = cidx // 2, cidx % 2
        out[b, p * TH:(p + 1) * TH] = res.results[cidx]['y']
    return out


def kernel(**inputs):
    cfg = Cfg()
    return run(cfg, inputs)
```

```python
import numpy as np
from contextlib import ExitStack
import concourse.bass as bass
import concourse.mybir as mybir
from concourse.bass_utils import run_bass_kernel_spmd

F32 = mybir.dt.float32
BF16 = mybir.dt.bfloat16
AF = mybir.ActivationFunctionType
ALU = mybir.AluOpType


class Cfg:
    def __init__(self, D=4096, T=2048, TH=1024, HA=16, HKV=2, HB=8, HC=8, DFF=16384, L=2, NCORES=8):
        self.D, self.T, self.TH, self.HA, self.HKV, self.HB, self.HC, self.DFF, self.L = D, T, TH, HA, HKV, HB, HC, DFF, L
        self.NCORES = NCORES
        self.GRP = HA // HKV
        self.A_Q, self.A_KV = HA * 64, HKV * 64
        self.B_QK, self.B_V, self.C_W = HB * 64, HB * 128, HC * 128
        widths = [self.A_Q, self.A_KV, self.A_KV, self.B_QK, self.B_QK, self.B_V, HB, HB, self.B_V,
                  self.C_W, self.C_W, self.C_W, HC, D, D, D]
        names = ['aq', 'ak', 'av', 'mq', 'mk', 'mv', 'mi', 'mf', 'mo', 'cq', 'ck', 'cv', 'cf', 'ga', 'gb', 'gc']
        self.off = {}
        o = 0
        for n, w in zip(names, widths):
            self.off[n] = (o, w)
            o += w
        self.D_IN = o
        self.DC = D // 128
        self.NB = T // 128
        self.NH = T // TH
        self.alpha = float((2 * L) ** 0.25)
        assert self.A_KV == 128 and TH % 512 == 0 and D % 512 == 0


class Buf:
    __slots__ = ("w", "wx", "r")

    def __init__(self):
        self.w = {}
        self.wx = {}
        self.r = {}


def _merge(d, s):
    for k, v in s.items():
        if d.get(k, 0) < v:
            d[k] = v


class KB:
    def __init__(self, nc, es):
        self.nc = nc
        self.es = es
        self.eng = {'pe': nc.tensor, 'act': nc.scalar, 'dve': nc.vector, 'pool': nc.gpsimd, 'sp': nc.sync}
        self.sems = {}
        self.cnt = {}
        for e in self.eng:
            self.sems[e] = es.enter_context(nc.semaphore("s_" + e))
            self.cnt[e] = 0
        self.seen = {e: {} for e in self.eng}
        self.rings = {}
        for q, n in (('sp', 12), ('pool', 6), ('act', 4)):
            self.rings[q] = [[es.enter_context(nc.semaphore(f"d_{q}{i}")), 0] for i in range(n)]
        self.ridx = {q: 0 for q in self.rings}
        self.semobj = dict(self.sems)
        for q, ring in self.rings.items():
            for i, (s, _) in enumerate(ring):
                self.semobj[f"d_{q}{i}"] = s

    def _waits(self, e, deps):
        eng = self.eng[e]
        seen = self.seen[e]
        for k, v in deps.items():
            if seen.get(k, 0) < v:
                eng.wait_ge(self.semobj[k], v)
                seen[k] = v

    def _deps(self, reads, writes, awrites):
        deps = {}
        for b in reads:
            _merge(deps, b.w)
        for b in writes:
            _merge(deps, b.w)
            _merge(deps, b.r)
        for b in awrites:
            _merge(deps, b.wx)
            _merge(deps, b.r)
        return deps

    def _record(self, key, val, reads, writes, awrites):
        for b in reads:
            if b.r.get(key, 0) < val:
                b.r[key] = val
        for b in writes:
            b.w = {key: val}
            b.wx = {key: val}
            b.r = {}
        for b in awrites:
            if b.w.get(key, 0) < val:
                b.w[key] = val

    def op(self, e, fn, reads=(), writes=(), awrites=()):
        self._waits(e, self._deps(reads, writes, awrites))
        ins = fn(self.eng[e])
        ins.then_inc(self.sems[e], 1)
        self.cnt[e] += 1
        self._record(e, self.cnt[e], reads, writes, awrites)

    def dma(self, q, out, in_, reads=(), writes=(), awrites=(), slow=False):
        self._waits(q, self._deps(reads, writes, awrites))
        ring = self.rings[q]
        i = self.ridx[q]
        self.ridx[q] = (i + 1) % len(ring)
        sem, c = ring[i]
        key = f"d_{q}{i}"
        if c > 0:
            self._waits(q, {key: 16 * c})
        (self.eng[q].dma_start(out=out, in_=in_, allow_slow_non_contiguous=True) if slow else self.eng[q].dma_start(out=out, in_=in_)).then_inc(sem, 16)
        ring[i][1] = c + 1
        self._record(key, 16 * (c + 1), reads, writes, awrites)

    def barrier(self):
        allv = {e: self.cnt[e] for e in self.eng if self.cnt[e] > 0}
        for q, ring in self.rings.items():
            for i, (s, c) in enumerate(ring):
                if c > 0:
                    allv[f"d_{q}{i}"] = 16 * c
        for e in self.eng:
            self._waits(e, allv)

    def final_wait(self):
        self.barrier()


def build_program(cfg, debug=False):
    nc = bass.Bass("TRN2", target_bir_lowering=False)
    c = cfg
    D, T, TH, DC, NB, L, DFF = c.D, c.T, c.TH, c.DC, c.NB, c.L, c.DFF
    NTT = TH // 128
    NTG = TH // 512

    def din(name, shape, dt=F32):
        return nc.dram_tensor(name, list(shape), dt, kind="ExternalInput").ap()

    def dscr(name, shape, dt):
        return nc.dram_tensor(name, list(shape), dt, kind=("ExternalOutput" if debug else "Internal")).ap()

    x_in = din("x", [TH, D])
    flags_in = din("flags", [1, 2])
    w_in = [din(f"w_in_{i}", [D, c.D_IN]) for i in range(L)]
    b_i = din("b_mlstm_i", [L, c.HB])
    b_f = din("b_mlstm_f", [L, c.HB])
    b_cf = din("b_fox_f", [L, c.HC])
    sinks = din("attn_sinks", [L, c.HA])
    w_up = [din("w_up_swa", [L, c.A_Q, D]), din("w_up_mlstm", [L, c.B_V, D]), din("w_up_fox", [L, c.C_W, D])]
    w_o = din("w_o", [L, D, D])
    ln1_g = din("ln1_g", [L, D]); ln1_b = din("ln1_b", [L, D])
    w_ff1 = [din(f"w_ff1_{i}", [D, DFF]) for i in range(L)]; w_ff2 = [din(f"w_ff2_{i}", [DFF, D]) for i in range(L)]
    ln2_g = din("ln2_g", [L, D]); ln2_b = din("ln2_b", [L, D])
    c_ident = din("c_ident", [128, 128])
    c_tri = din("c_tri", [128, 128])
    c_alibi = din("c_alibi", [128, c.HA * 256])
    y_out = nc.dram_tensor("y", [TH, D], F32, kind="ExternalOutput").ap()

    xT_scr = dscr("xT_scr", [D, TH], BF16)
    R_FM = c.A_Q + 256 + 2 * c.B_QK + 2 * c.C_W
    o_qa, o_ka, o_mq = 0, c.A_Q, c.A_Q + 256
    o_mk = o_mq + c.B_QK
    o_cq = o_mk + c.B_QK
    o_ck = o_cq + c.C_W
    C_TM = 128 + c.B_V + c.C_W
    NG = 2 * c.HB + c.HC
    FMs = dscr("FMs", [R_FM, TH], BF16)
    TMs = dscr("TMs", [TH, C_TM], BF16)
    CHF, CHT = 512, 256
    fm_rows = [min(CHF, R_FM - k0) for k0 in range(0, R_FM, CHF)]
    FMg_l = [dscr(f"FMg{k}", [2 * rk, TH], BF16) for k, rk in enumerate(fm_rows)]
    TMg_l = [dscr(f"TMg{k}", [2 * CHT, C_TM], BF16) for k in range(TH // CHT)]

    def fm_g(r, row0, n=128):
        k, i = row0 // CHF, row0 % CHF
        return FMg_l[k][r * fm_rows[k] + i:r * fm_rows[k] + i + n, :]

    def tm_g(jb):
        r, lb = jb // (TH // 128), jb % (TH // 128)
        k, ii = lb // (CHT // 128), (lb % (CHT // 128)) * 128
        return TMg_l[k][r * CHT + ii:r * CHT + ii + 128, :]
    gifs = dscr("gifs", [TH, NG], F32); gifg = dscr("gifg", [2 * TH, NG], F32)
    qaT = FMs[o_qa:o_qa + c.A_Q, :]; kaT = FMs[o_ka:o_ka + 256, :]
    mqT = FMs[o_mq:o_mq + c.B_QK, :]; mkT = FMs[o_mk:o_mk + c.B_QK, :]
    cqT = FMs[o_cq:o_cq + c.C_W, :]; ckT = FMs[o_ck:o_ck + c.C_W, :]
    va = TMs[:, 0:128]; mv = TMs[:, 128:128 + c.B_V]; cv = TMs[:, 128 + c.B_V:C_TM]
    gif = gifs
    moT = dscr("moT", [c.B_V, TH], BF16)
    gT = dscr("gT", [3 * D, TH], BF16)
    yT = dscr("yT", [c.A_Q + c.B_V + c.C_W, T], BF16)
    r_scr = dscr("r_scr", [TH, D], F32)
    x1_scr = dscr("x1_scr", [TH, D], F32)
    x2_scr = dscr("x2_scr", [TH, D], F32)
    hidT = dscr("hidT", [DFF, TH], BF16)
    PAIRS = [[2 * i, 2 * i + 1] for i in range(c.NCORES // 2)]
    dbg = {}

    es = ExitStack()
    with es:
        es.enter_context(nc.allow_low_precision("bf16 matmul operands, fp32 accumulation"))
        kb = KB(nc, es)

        sbctr = [0]

        def sb(name, shape, dt):
            sbctr[0] += 1
            return es2.enter_context(nc.sbuf_tensor(f"{name}_{sbctr[0]}", list(shape), dt))

        es2 = es
        ident = sb("ident", [128, 128], F32)
        tri_f = sb("tri_f", [128, 128], F32)
        tri_b = sb("tri_b", [128, 128], BF16)
        ones_f = sb("ones_f", [128, 128], F32)
        ones_b = sb("ones_b", [128, 128], BF16)
        B_const = Buf()
        psum = [es.enter_context(nc.psum_tensor(f"ps{i}", [128, 512], F32)) for i in range(8)]
        PB = [Buf() for _ in range(8)]

        fl = sb("flags", [128, 2], F32)
        kb.dma('sp', fl[:], bass.AP(flags_in.tensor, flags_in.offset, [[0, 128], [1, 2]]), awrites=[B_const])
        kb.dma('sp', ident[:], c_ident, awrites=[B_const])
        kb.dma('sp', tri_f[:], c_tri, awrites=[B_const])
        kb.op('dve', lambda e: e.tensor_copy(out=tri_b[:], in_=tri_f[:]), reads=[B_const], awrites=[B_const])
        ident_b = sb("ident_b", [128, 128], BF16)
        kb.op('dve', lambda e: e.tensor_copy(out=ident_b[:], in_=ident[:]), reads=[B_const], awrites=[B_const])
        psum_b = [p[:].bitcast(BF16) for p in psum]
        kb.op('dve', lambda e: e.memset(ones_f[:], 1.0), awrites=[B_const])
        kb.op('dve', lambda e: e.memset(ones_b[:], 1.0), awrites=[B_const])
        kb.barrier()

        class TransposeCtx:
            def __init__(self):
                self.i = 0

        tctx = TransposeCtx()

        def transpose_tile(src, src_buf, dst_fn, dst_buf, dst_mode):
            for cq in range(DC // 4):
                bi = tctx.i % 4
                tctx.i += 1
                ps, pb = psum_b[bi], PB[bi]

                def mm(e, cq=cq, ps=ps):
                    ins = None
                    for k in range(4):
                        ins = e.transpose(out=ps[:, k * 128:(k + 1) * 128], in_=src[:, (cq * 4 + k) * 128:(cq * 4 + k + 1) * 128], identity=ident_b[:])
                    return ins
                kb.op('pe', mm, reads=[src_buf, B_const], writes=[pb])
                eng = 'act' if (cq % 2 == 0) else 'dve'
                d = dst_fn(cq)
                if eng == 'act':
                    kb.op('act', lambda e, d=d, ps=ps: e.activation(out=d, in_=ps[:, 0:512].rearrange("p (k t) -> p k t", k=4), func=AF.Copy),
                          reads=[pb], **{dst_mode: [dst_buf]})
                else:
                    kb.op('dve', lambda e, d=d, ps=ps: e.tensor_copy(out=d, in_=ps[:, 0:512].rearrange("p (k t) -> p k t", k=4)),
                          reads=[pb], **{dst_mode: [dst_buf]})

        xT_v = xT_scr.rearrange("(c p) t -> p c t", p=128)
        B_xT = Buf()

        def to_xT_scr(src_dram, B_src):
            with ExitStack() as es_l:
                nonlocal es2
                es2_old = es2
                es2 = es_l
                xt = [sb(f"p0_xt{i}", [128, D], F32) for i in range(2)]
                Bx = [Buf(), Buf()]
                st = [sb(f"p0_st{i}", [128, DC, 128], BF16) for i in range(2)]
                Bs = [Buf(), Buf()]
                xb = [sb(f"p0_xb{i}", [128, D], BF16) for i in range(2)]
                Bxb = [Buf(), Buf()]
                for tt in range(TH // 128):
                    i = tt % 2
                    kb.dma('sp', xt[i][:], src_dram[tt * 128:(tt + 1) * 128, :], reads=[B_src], writes=[Bx[i]])
                    kb.op('act', lambda e, i=i: e.activation(out=xb[i][:], in_=xt[i][:], func=AF.Copy), reads=[Bx[i]], writes=[Bxb[i]])
                    transpose_tile(xb[i], Bxb[i], lambda cq, i=i: st[i][:, cq * 4:(cq + 1) * 4, :], Bs[i], 'awrites')
                    kb.dma('act', xT_v[:, :, tt * 128:(tt + 1) * 128], st[i][:], reads=[Bs[i]], awrites=[B_xT])
                    kb.op('dve', lambda e: e.engine_nop(), writes=[Bs[i]]) if False else None
                    Bs[i].wx = dict(Bs[i].w)
                kb.barrier()
                es2 = es2_old

        B_xin = Buf()
        to_xT_scr(x_in, B_xin)

        def gemm_half(actT, B_act, KC, blocks, wbufs, Bw, wctr):
            for blk in blocks:
                wi = wctr[0] % len(wbufs)
                wctr[0] += 1
                wb, bw = wbufs[wi], Bw[wi]
                loads = blk['wload'](wb)
                first = True
                for (o_ap, i_ap) in loads:
                    if first:
                        kb.dma('pool', o_ap, i_ap, writes=[bw])
                        first = False
                    else:
                        kb.dma('pool', o_ap, i_ap, awrites=[bw])
                if 'before_mm' in blk:
                    blk['before_mm']()
                ncols = blk['ncols']
                if blk['orient'] == 'F':
                    ncc = ncols // 128
                    bidx = 0
                    for cc in range(ncc):
                        for tg in range(NTG):
                            b = blk.get('bank0', 0) + bidx
                            bidx += 1
                            ps, pb = psum[b % 8], PB[b % 8]

                            def mm(e, cc=cc, tg=tg, ps=ps):
                                ins = None
                                for k in range(KC):
                                    ins = e.matmul(ps[:, :], lhsT=wb[:, k, cc * 128:(cc + 1) * 128], rhs=actT[:, k, tg * 512:(tg + 1) * 512],
                                                   start=(k == 0), stop=(k == KC - 1))
                                return ins
                            kb.op('pe', mm, reads=[bw, B_act], writes=[pb])
                            blk['epi'](cc, tg, ps, pb)
                else:
                    for tt in range(NTT):
                        ps, pb = psum[tt % 8], PB[tt % 8]

                        def mm(e, tt=tt, ps=ps):
                            ins = None
                            for k in range(KC):
                                ins = e.matmul(ps[:, :ncols], lhsT=actT[:, k, tt * 128:(tt + 1) * 128], rhs=wb[:, k, :ncols],
                                               start=(k == 0), stop=(k == KC - 1))
                            return ins
                        kb.op('pe', mm, reads=[bw, B_act], writes=[pb])
                        blk['epi'](tt, ps, pb)

        def wview(w2d, k0, kc, c0, ncols):
            return w2d[k0 * 128:(k0 + kc) * 128, c0:c0 + ncols].rearrange("(c p) n -> p c n", p=128)

        class Ring:
            def __init__(self, tiles):
                self.t = tiles
                self.b = [Buf() for _ in tiles]
                self.i = 0

            def next(self):
                i = self.i % len(self.t)
                self.i += 1
                return self.t[i], self.b[i]

        epi_ctr = [0]

        def epi_engine():
            epi_ctr[0] += 1
            return 'act' if epi_ctr[0] % 2 == 0 else 'dve'

        B_w2b = Buf()
        x_res = x_in
        B_xres = B_xin
        for l in range(L):
            B_scr = {n: Buf() for n in ['qaT', 'kaT', 'va', 'mqT', 'mkT', 'mv', 'moT', 'cqT', 'ckT', 'cv', 'gif', 'gT', 'yT', 'r', 'x1', 'x2', 'hid']}
            with ExitStack() as es_l:
                es2 = es_l
                actA = sb("A_act", [128, DC, TH], BF16)
                B_A = Buf()
                wbufs = [sb(f"A_w{i}", [128, DC, 512], BF16) for i in range(2)]
                Bw = [Buf(), Buf()]
                wctr = [0]
                stb = Ring([sb(f"A_sb{i}", [128, 512], BF16) for i in range(6)])
                stf = Ring([sb(f"A_sf{i}", [128, 32], F32) for i in range(4)])
                W = w_in[l]
                for hf in range(1):
                    t0 = 0
                    kb.dma('sp', actA[:], xT_v[:, :, t0:t0 + TH], reads=[B_xT], writes=[B_A])
                    blocks = []

                    def mk_F(seg, dst, func, scale, dst_row0=0, swap=False):
                        o, wd = c.off[seg]
                        for c0 in range(0, wd, 512):
                            ncols = min(512, wd - c0)

                            def wload(wb, o=o, c0=c0, ncols=ncols, swap=swap):
                                if not swap:
                                    return [(wb[:, :, :ncols], wview(W, 0, DC, o + c0, ncols))]
                                return [(wb[:, :, 0:64], wview(W, 0, DC, o + 64, 64)), (wb[:, :, 64:128], wview(W, 0, DC, o, 64))]

                            def epi(cc, tg, ps, pb, c0=c0, dst=dst, func=func, scale=scale, dst_row0=dst_row0, seg=seg):
                                st, bs = stb.next()
                                kb.op('act', lambda e: e.activation(out=st[:, :], in_=ps[:, :], func=func, scale=scale), reads=[pb], writes=[bs])
                                r0 = dst_row0 + c0 + cc * 128
                                kb.dma('sp', dst[0][r0:r0 + 128, t0 + tg * 512:t0 + (tg + 1) * 512], st[:, :], reads=[bs], awrites=[B_scr[dst[1]]])
                            blocks.append(dict(wload=wload, ncols=ncols, orient='F', epi=epi))

                    def mk_T(seg, dst, dcol0, fp32=False, seg2=None):
                        o, wd = c.off[seg]
                        if seg2 is not None:
                            wd += c.off[seg2][1]
                        for c0 in range(0, wd, 512):
                            ncols = min(512, wd - c0)

                            def wload(wb, o=o, c0=c0, ncols=ncols):
                                return [(wb[:, :, :ncols], wview(W, 0, DC, o + c0, ncols))]

                            def epi(tt, ps, pb, c0=c0, ncols=ncols, dst=dst, dcol0=dcol0, fp32=fp32):
                                if fp32:
                                    st, bs = stf.next()
                                else:
                                    st, bs = stb.next()
                                eng = epi_engine()
                                if eng == 'act':
                                    kb.op('act', lambda e: e.activation(out=st[:, :ncols], in_=ps[:, :ncols], func=AF.Copy), reads=[pb], writes=[bs])
                                else:
                                    kb.op('dve', lambda e: e.tensor_copy(out=st[:, :ncols], in_=ps[:, :ncols]), reads=[pb], writes=[bs])
                                kb.dma('sp', dst[0][t0 + tt * 128:t0 + (tt + 1) * 128, dcol0 + c0:dcol0 + c0 + ncols], st[:, :ncols],
                                       reads=[bs], awrites=[B_scr[dst[1]]])
                            blocks.append(dict(wload=wload, ncols=ncols, orient='T', epi=epi))

                    mk_F('aq', (qaT, 'qaT'), AF.Copy, 0.125)
                    mk_F('ak', (kaT, 'kaT'), AF.Copy, 1.0)
                    mk_F('ak', (kaT, 'kaT'), AF.Copy, 1.0, dst_row0=128, swap=True)
                    mk_T('av', (va, 'va'), 0)
                    mk_F('mq', (mqT, 'mqT'), AF.Copy, 1.0)
                    mk_F('mk', (mkT, 'mkT'), AF.Copy, 0.125)
                    mk_T('mv', (mv, 'mv'), 0)
                    mk_T('mi', (gif, 'gif'), 0, fp32=True, seg2='mf')
                    mk_F('cq', (cqT, 'cqT'), AF.Copy, float(128 ** -0.5))
                    mk_F('ck', (ckT, 'ckT'), AF.Copy, 1.0)
                    mk_T('cv', (cv, 'cv'), 0)
                    mk_T('cf', (gif, 'gif'), 2 * c.HB, fp32=True)
                    n_mix_blocks = len(blocks)
                    mk_F('mo', (moT, 'moT'), AF.Sigmoid, 1.0)
                    mk_F('ga', (gT, 'gT'), AF.Sigmoid, 1.0, dst_row0=0)
                    mk_F('gb', (gT, 'gT'), AF.Sigmoid, 1.0, dst_row0=D)
                    mk_F('gc', (gT, 'gT'), AF.Sigmoid, 1.0, dst_row0=2 * D)

                    cc_list = [(gifs[:, :], gifg[:, :])]
                    for k, rk in enumerate(fm_rows):
                        cc_list.append((FMs[k * CHF:k * CHF + rk, :], FMg_l[k][:, :]))
                    for k in range(TH // CHT):
                        cc_list.append((TMs[k * CHT:(k + 1) * CHT, :], TMg_l[k][:, :]))
                    cc_sems = [es.enter_context(nc.semaphore(f"cc_{l}_{i}")) for i in range(len(cc_list))]

                    def issue_cc():
                        deps = {}
                        for n in ['qaT', 'kaT', 'va', 'mqT', 'mkT', 'mv', 'cqT', 'ckT', 'cv', 'gif']:
                            _merge(deps, B_scr[n].w)
                        kb._waits('pool', deps)
                        for i, (src_, dst_) in enumerate(cc_list):
                            nc.gpsimd.collective_compute("AllGather", ALU.bypass, replica_groups=PAIRS, ins=[src_], outs=[dst_]).then_inc(cc_sems[i], 1)
                    blocks[n_mix_blocks]['before_mm'] = issue_cc
                    gemm_half(actA, B_A, DC, blocks, wbufs, Bw, wctr)
                kb.barrier()
            for e_name in kb.eng:
                for i in range(len(cc_list)):
                    kb.eng[e_name].wait_ge(cc_sems[i], 1)
            NGH = 2 * c.HB + c.HC
            NFH = c.HB + c.HC
            with ExitStack() as es_l:
                es2 = es_l
                Gs = sb("B_Gs", [128, NB, NGH], F32)
                bcat = sb("B_bcat", [128, NGH], F32)
                nlf = sb("B_nlf", [128, NB, NFH], F32)
                negB = sb("B_negB", [128, NB, NFH], F32)
                a_m = sb("B_am", [128, NB, c.HB], F32)
                B_g = Buf()
                kb.dma('sp', Gs[:], gifg.rearrange("(j p) g -> p j g", p=128), reads=[B_scr['gif']], writes=[B_g])

                def bc_row(src1d, n):
                    return bass.AP(src1d.tensor, src1d.offset, [[0, 128], [1, n]])
                kb.dma('sp', bcat[:, 0:c.HB], bc_row(b_i[l], c.HB), awrites=[B_g])
                kb.dma('sp', bcat[:, c.HB:2 * c.HB], bc_row(b_f[l], c.HB), awrites=[B_g])
                kb.dma('sp', bcat[:, 2 * c.HB:NGH], bc_row(b_cf[l], c.HC), awrites=[B_g])
                B_g2 = Buf()

                def addb(e):
                    ins = None
                    for j in range(NB):
                        ins = e.tensor_tensor(out=Gs[:, j, :], in0=Gs[:, j, :], in1=bcat[:, :], op=ALU.add)
                    return ins
                kb.op('dve', addb, reads=[B_g], writes=[B_g2])
                kb.op('act', lambda e: e.activation(out=nlf[:], in_=Gs[:, :, c.HB:NGH], func=AF.Exp, scale=-1.0), reads=[B_g2], writes=[B_g])
                B_nlf = Buf()
                kb.op('act', lambda e: e.activation(out=nlf[:], in_=nlf[:], func=AF.Ln, bias=1.0, scale=1.0), reads=[B_g], writes=[B_nlf])
                B_negB = Buf()
                for j in range(NB):
                    ps, pb = psum[j % 4], PB[j % 4]

                    def mm(e, j=j, ps=ps):
                        ins = e.matmul(ps[:, :NFH], lhsT=tri_f[:], rhs=nlf[:, j, :], start=True, stop=(j == 0))
                        for jj in range(j):
                            ins = e.matmul(ps[:, :NFH], lhsT=ones_f[:], rhs=nlf[:, jj, :], start=False, stop=(jj == j - 1))
                        return ins
                    kb.op('pe', mm, reads=[B_nlf, B_const], writes=[pb])
                    kb.op('dve', lambda e, j=j, ps=ps: e.tensor_copy(out=negB[:, j, :], in_=ps[:, :NFH]), reads=[pb], awrites=[B_negB])
                B_am = Buf()
                kb.op('dve', lambda e: e.tensor_tensor(out=a_m[:], in0=Gs[:, :, 0:c.HB], in1=negB[:, :, 0:c.HB], op=ALU.add),
                      reads=[B_g2, B_negB], writes=[B_am])

                brow = [sb(f"B_brow{i}", [128, T], F32) for i in range(2)]
                B_brow = [Buf(), Buf()]
                bbt = Ring([sb(f"B_bb{i}", [128, 128], F32) for i in range(3)])

                def make_brow(hd, slot):
                    for j4 in range(NB // 4):
                        ps, pb = psum[0], PB[0]
                        for k in range(4):
                            j = j4 * 4 + k
                            bb, bbb = bbt.next()
                            kb.op('dve', lambda e, j=j, bb=bb: e.tensor_scalar_mul(out=bb[:], in0=ones_f[:], scalar1=negB[:, j, hd:hd + 1]), reads=[B_negB, B_const], writes=[bbb])
                            kb.op('pe', lambda e, k=k, bb=bb, ps=ps: e.matmul(ps[:, k * 128:(k + 1) * 128], lhsT=bb[:], rhs=ident[:], start=True, stop=True),
                                  reads=[bbb, B_const], **({'writes': [pb]} if k == 0 else {'awrites': [pb]}))
                        kb.op('act', lambda e, j4=j4, ps=ps: e.activation(out=brow[slot][:, j4 * 512:(j4 + 1) * 512], in_=ps[:, :], func=AF.Copy, scale=-1.0),
                              reads=[pb], **({'writes': [B_brow[slot]]} if j4 == 0 else {'awrites': [B_brow[slot]]}))

                qh = [sb(f"B_qh{i}", [128, T], BF16) for i in range(2)]
                kh = [sb(f"B_kh{i}", [128, T], BF16) for i in range(2)]
                vh = [sb(f"B_vh{i}", [128, NB, 128], BF16) for i in range(2)]
                B_qkv = [Buf(), Buf()]
                pTr = Ring([sb(f"B_pT{i}", [128, 512], BF16) for i in range(5)])
                wTr = Ring([sb(f"B_wT{i}", [128, 512], F32) for i in range(4)])
                crow_b = [sb(f"B_crow{i}", [1, T], BF16) for i in range(2)]
                B_crow = [Buf(), Buf()]
                rlr = Ring([sb(f"B_rl{i}", [128, 512], F32) for i in range(2)])
                yst = Ring([sb(f"B_yst{i}", [128, 512], BF16) for i in range(3)])
                NTG4 = T // 512
                heads = [('mlstm', h) for h in range(c.HB)] + [('fox', h) for h in range(c.HC)]

                def head_params(kind, h):
                    if kind == 'fox':
                        return dict(hd=c.HB + h, qrow=o_cq + h * 128, krow=o_ck + h * 128, vc0=128 + c.B_V + h * 128, KP=128, p0=0,
                                    yrow=c.A_Q + c.B_V + h * 128)
                    return dict(hd=h, qrow=o_mq + (h // 2) * 128, krow=o_mk + (h // 2) * 128, vc0=128 + h * 128, KP=64, p0=(h % 2) * 64,
                                yrow=c.A_Q + h * 128)

                def load_head(idx):
                    kind, h = heads[idx]
                    slot = idx % 2
                    hp = head_params(kind, h)
                    first = True
                    for dst_t, row0 in ((qh[slot], hp['qrow']), (kh[slot], hp['krow'])):
                        for r in range(2):
                            kb.dma('sp', dst_t[:, r * TH:(r + 1) * TH], fm_g(r, row0), **({'writes': [B_qkv[slot]]} if first else {'awrites': [B_qkv[slot]]}))
                            first = False
                    for jb in range(NB):
                        kb.dma('sp', vh[slot][:, jb, :], tm_g(jb)[:, hp['vc0']:hp['vc0'] + 128], awrites=[B_qkv[slot]])
                    make_brow(hp['hd'], slot)
                    if kind == 'fox':
                        kb.op('act', lambda e: e.activation(out=crow_b[slot][0:1, :], in_=brow[slot][0:1, :], func=AF.Copy), reads=[B_brow[slot]], writes=[B_crow[slot]])

                def compute_head(idx):
                    kind, h = heads[idx]
                    slot = idx % 2
                    hp = head_params(kind, h)
                    hd, KP, p0 = hp['hd'], hp['KP'], hp['p0']
                    q_, k_, v_, br = qh[slot], kh[slot], vh[slot], brow[slot]
                    iters = [(tg, sbk) for tg in range(NTG4) for sbk in range(4 * tg + 4)]
                    state = {}

                    def stageA(ii):
                        tg, sbk = iters[ii]
                        d = sbk - 4 * tg
                        c0 = max(d, 0) * 128
                        psS, pbS = psum[1 + ii % 3], PB[1 + ii % 3]
                        tcol = slice(tg * 512 + c0, (tg + 1) * 512)
                        pT, bpT = pTr.next()
                        if kind == 'fox':
                            def mm(e):
                                e.matmul(psS[:, c0:], lhsT=k_[:, sbk * 128:(sbk + 1) * 128], rhs=q_[:, tcol], start=True, stop=False)
                                return e.matmul(psS[:, c0:], lhsT=ones_b[0:1, :], rhs=crow_b[slot][0:1, tcol], start=False, stop=True)
                            kb.op('pe', mm, reads=[B_qkv[slot], B_crow[slot], B_const], writes=[pbS])
                            kb.op('act', lambda e: e.activation(out=pT[:, c0:], in_=psS[:, c0:], func=AF.Exp, bias=negB[:, sbk, hd:hd + 1], scale=1.0),
                                  reads=[pbS, B_negB], writes=[bpT])
                        else:
                            kb.op('pe', lambda e: e.matmul(psS[:, c0:], lhsT=k_[p0:p0 + KP, sbk * 128:(sbk + 1) * 128], rhs=q_[p0:p0 + KP, tcol], start=True, stop=True),
                                  reads=[B_qkv[slot]], writes=[pbS])
                            wT, bwT = wTr.next()
                            kb.op('act', lambda e: e.activation(out=wT[:, c0:], in_=br[:, tcol], func=AF.Exp, bias=a_m[:, sbk, hd:hd + 1], scale=1.0),
                                  reads=[B_brow[slot], B_am], writes=[bwT])
                            kb.op('dve', lambda e: e.tensor_tensor(out=pT[:, c0:], in0=psS[:, c0:], in1=wT[:, c0:], op=ALU.mult),
                                  reads=[pbS, bwT], writes=[bpT])
                        if d >= 0:
                            if c0 > 0:
                                kb.op('dve', lambda e: e.memset(pT[:, 0:c0], 0.0), awrites=[bpT])
                            kb.op('dve', lambda e: e.tensor_tensor(out=pT[:, c0:c0 + 128], in0=pT[:, c0:c0 + 128], in1=tri_b[:], op=ALU.mult),
                                  reads=[bpT, B_const], writes=[bpT])
                        state[ii] = (pT, bpT)

                    def stageB(ii):
                        tg, sbk = iters[ii]
                        nsb = 4 * tg + 4
                        psO, pbO = psum[4 + (tg % 2)], PB[4 + (tg % 2)]
                        psL, pbL = psum[6 + (tg % 2)], PB[6 + (tg % 2)]
                        pT, bpT = state.pop(ii)
                        first, last = (sbk == 0), (sbk == nsb - 1)
                        kb.op('pe', lambda e: e.matmul(psO[:, :], lhsT=v_[:, sbk, :], rhs=pT[:, :], start=first, stop=last),
                              reads=[bpT, B_qkv[slot]], **({'writes': [pbO]} if first else {'awrites': [pbO]}))
                        kb.op('pe', lambda e: e.matmul(psL[:, :], lhsT=ones_b[:], rhs=pT[:, :], start=first, stop=last),
                              reads=[bpT, B_const], **({'writes': [pbL]} if first else {'awrites': [pbL]}))
                        if last:
                            yst_t, byst = yst.next()
                            rl, B_rl = rlr.next()
                            if kind == 'fox':
                                kb.op('dve', lambda e: e.reciprocal(out=rl[:], in_=psL[:, :]), reads=[pbL], writes=[B_rl])
                            else:
                                kb.op('act', lambda e: e.activation(out=rl[:], in_=psL[:, :], func=AF.Abs), reads=[pbL], writes=[B_rl])
                                kb.op('dve', lambda e: e.tensor_scalar_max(out=rl[:], in0=rl[:], scalar1=1.0), reads=[B_rl], writes=[B_rl])
                                kb.op('dve', lambda e: e.reciprocal(out=rl[:], in_=rl[:]), reads=[B_rl], writes=[B_rl])
                            kb.op('dve', lambda e: e.tensor_tensor(out=yst_t[:], in0=psO[:, :], in1=rl[:], op=ALU.mult), reads=[pbO, B_rl], writes=[byst])
                            kb.dma('sp', yT[hp['yrow']:hp['yrow'] + 128, tg * 512:(tg + 1) * 512], yst_t[:], reads=[byst], awrites=[B_scr['yT']])

                    stageA(0)
                    stageA(1)
                    for ii in range(len(iters)):
                        if ii + 2 < len(iters):
                            stageA(ii + 2)
                        stageB(ii)

                NJ = c.HA // 2
                qT_all = sb("S_q", [128, NJ, T], BF16)
                kT2 = sb("S_k", [128, 2, T], BF16)
                vraw = sb("S_vraw", [128, NB, 128], BF16)
                Vp = [[sb(f"S_V{g}{e}", [128, NB, 128], BF16) for e in range(2)] for g in range(2)]
                ones_lo = sb("S_olo", [128, 128], BF16)
                ones_hi = sb("S_ohi", [128, 128], BF16)
                Etab = sb("S_E", [128, c.HA, 256], F32)
                esink = sb("S_es", [128, NJ], F32)
                B_s = Buf()
                first_ = True
                for r in range(2):
                    for j in range(NJ):
                        kb.dma('sp', qT_all[:, j, r * TH:(r + 1) * TH], fm_g(r, o_qa + j * 128), **({'writes': [B_s]} if first_ else {'awrites': [B_s]}))
                        first_ = False
                    for j in range(2):
                        kb.dma('sp', kT2[:, j, r * TH:(r + 1) * TH], fm_g(r, o_ka + j * 128), awrites=[B_s])
                for jb in range(NB):
                    kb.dma('sp', vraw[:, jb, :], tm_g(jb)[:, 0:128], awrites=[B_s])
                kb.dma('sp', Etab[:], c_alibi.rearrange("p (h c) -> p h c", c=256), awrites=[B_s])
                sk = sinks[l]
                for e_ in range(2):
                    kb.dma('sp', esink[e_ * 64:(e_ + 1) * 64, :], bass.AP(sk.tensor, sk.offset + e_, [[0, 64], [2, NJ]]), awrites=[B_s], slow=True)
                B_s2 = Buf()

                kb.op('dve', lambda e: e.memset(ones_lo[:], 0.0), writes=[B_s2])
                kb.op('dve', lambda e: e.memset(ones_hi[:], 0.0), reads=[B_s2], writes=[B_s2])
                kb.op('dve', lambda e: e.memset(ones_lo[:, 0:64], 1.0), reads=[B_s2], writes=[B_s2])
                kb.op('dve', lambda e: e.memset(ones_hi[:, 64:128], 1.0), reads=[B_s2], writes=[B_s2])
                for g in range(2):
                    for e_ in range(2):
                        kb.op('dve', lambda e, g=g, e_=e_: e.memset(Vp[g][e_][:], 0.0), reads=[B_s2], writes=[B_s2])

                def prep2(e):
                    ins = None
                    for g in range(2):
                        e.tensor_copy(out=Vp[g][0][:, :, 0:64], in_=vraw[:, :, g * 64:(g + 1) * 64])
                        ins = e.tensor_copy(out=Vp[g][1][:, :, 64:128], in_=vraw[:, :, g * 64:(g + 1) * 64])
                    return ins
                kb.op('dve', prep2, reads=[B_s, B_s2], writes=[B_s2])
                kb.op('act', lambda e: e.activation(out=esink[:], in_=esink[:], func=AF.Exp), reads=[B_s], writes=[B_s])
                load_head(0)
                for idx in range(len(heads)):
                    if idx + 1 < len(heads):
                        load_head(idx + 1)
                    compute_head(idx)
                pex = Ring([sb(f"S_pex{i}", [128, 256], F32) for i in range(4)])
                pTs = Ring([sb(f"S_pT{i}", [128, 256], BF16) for i in range(6)])
                lsr = Ring([sb(f"S_ls{i}", [128, 128], F32) for i in range(2)])
                ysts = Ring([sb(f"S_yst{i}", [128, 128], BF16) for i in range(3)])
                sw_its = [(qb, j) for qb in range(NB) for j in range(NJ)]
                sw_state = {}

                def swA(it):
                    qb, j = sw_its[it]
                    qs = slice(qb * 128, (qb + 1) * 128)
                    cl = 128 if qb == 0 else 0
                    lst = []
                    for e_ in range(2):
                        h = 2 * j + e_
                        g = h // c.GRP
                        kc = 0 if g == e_ else 1
                        pp = slice(e_ * 64, (e_ + 1) * 64)
                        psS, pbS = psum[(2 * it + e_) % 4], PB[(2 * it + e_) % 4]

                        def mm(e, psS=psS, pp=pp, kc=kc):
                            ins = e.matmul(psS[:, 128:256], lhsT=kT2[pp, kc, qs], rhs=qT_all[pp, j, qs], start=True, stop=True)
                            if qb > 0:
                                ins = e.matmul(psS[:, 0:128], lhsT=kT2[pp, kc, (qb - 1) * 128:qb * 128], rhs=qT_all[pp, j, qs], start=True, stop=True)
                            return ins
                        kb.op('pe', mm, reads=[B_s], writes=[pbS])
                        px, bpx = pex.next()
                        kb.op('act', lambda e, px=px, psS=psS: e.activation(out=px[:, cl:], in_=psS[:, cl:256], func=AF.Exp), reads=[pbS], writes=[bpx])
                        pT, bpT = pTs.next()
                        kb.op('dve', lambda e, px=px, pT=pT, h=h: e.tensor_tensor(out=pT[:, cl:], in0=px[:, cl:], in1=Etab[:, h, cl:], op=ALU.mult),
                              reads=[bpx, B_s], writes=[bpT])
                        lst.append((pT, bpT, g, e_))
                    sw_state[it] = lst

                def swB(it):
                    qb, j = sw_its[it]
                    qs = slice(qb * 128, (qb + 1) * 128)
                    psO, pbO = psum[4 + (it % 2)], PB[4 + (it % 2)]
                    psL, pbL = psum[6 + (it % 2)], PB[6 + (it % 2)]
                    nmm = 0
                    tot = 2 * (1 if qb == 0 else 2)
                    for (pT, bpT, g, e_) in sw_state.pop(it):
                        for kbk in ((1,) if qb == 0 else (0, 1)):
                            first, last = (nmm == 0), (nmm == tot - 1)
                            nmm += 1
                            vb = qb - 1 + kbk
                            cs = slice(kbk * 128, (kbk + 1) * 128)
                            kb.op('pe', lambda e, pT=pT, g=g, e_=e_, vb=vb, cs=cs, first=first, last=last:
                                  e.matmul(psO[:, 0:128], lhsT=Vp[g][e_][:, vb, :], rhs=pT[:, cs], start=first, stop=last),
                                  reads=[bpT, B_s2], **({'writes': [pbO]} if first else {'awrites': [pbO]}))
                            kb.op('pe', lambda e, pT=pT, e_=e_, cs=cs, first=first, last=last:
                                  e.matmul(psL[:, 0:128], lhsT=(ones_lo if e_ == 0 else ones_hi)[:], rhs=pT[:, cs], start=first, stop=last),
                                  reads=[bpT, B_s2], **({'writes': [pbL]} if first else {'awrites': [pbL]}))
                    ls, B_ls = lsr.next()
                    kb.op('dve', lambda e: e.tensor_scalar_add(out=ls[:], in0=psL[:, 0:128], scalar1=esink[:, j:j + 1]), reads=[pbL, B_s], writes=[B_ls])
                    kb.op('dve', lambda e: e.reciprocal(out=ls[:], in_=ls[:]), reads=[B_ls], writes=[B_ls])
                    ys, bys = ysts.next()
                    kb.op('dve', lambda e: e.tensor_tensor(out=ys[:], in0=psO[:, 0:128], in1=ls[:], op=ALU.mult), reads=[pbO, B_ls], writes=[bys])
                    kb.dma('sp', yT[j * 128:(j + 1) * 128, qs], ys[:], reads=[bys], awrites=[B_scr['yT']])

                swA(0)
                for it in range(len(sw_its)):
                    if it + 1 < len(sw_its):
                        swA(it + 1)
                    swB(it)
                kb.barrier()


            x_dst = y_out if l == L - 1 else x2_scr
            B_xdst = Buf()
            for hf in range(1):
                t0 = 0
                with ExitStack() as es_o:
                    es2 = es_o
                    actA = sb("C_act", [128, DC, TH], BF16)
                    B_A = Buf()
                    with ExitStack() as es_l:
                        es2 = es_l
                        KU = [c.A_Q // 128, c.B_V // 128, c.C_W // 128]
                        yrow = [0, c.A_Q, c.A_Q + c.B_V]
                        ybuf = [sb(f"C_y{i}", [128, KU[i], TH], BF16) for i in range(3)]
                        B_y = Buf()
                        ytmp = sb("C_ytmp", [128, max(KU), TH], BF16)
                        B_yt = Buf()
                        for br_ in range(3):
                            yv = yT[yrow[br_]:yrow[br_] + KU[br_] * 128, :].rearrange("(k p) t -> p k t", p=128)
                            yb_, yt_ = ybuf[br_], ytmp[:, :KU[br_], :]
                            kb.dma('sp', yb_[:], yv[:, :, 0:TH], reads=[B_scr['yT']], **({'writes': [B_y]} if br_ == 0 else {'awrites': [B_y]}))
                            kb.dma('sp', yt_, yv[:, :, TH:2 * TH], reads=[B_scr['yT']], writes=[B_yt])
                            kb.op('dve', lambda e, yb_=yb_: e.tensor_scalar_mul(out=yb_[:], in0=yb_[:], scalar1=fl[:, 0:1]), reads=[B_y, B_const], writes=[B_y])
                            kb.op('dve', lambda e, yb_=yb_, yt_=yt_: e.scalar_tensor_tensor(out=yb_[:], in0=yt_, scalar=fl[:, 1:2], in1=yb_[:], op0=ALU.mult, op1=ALU.add),
                                  reads=[B_y, B_yt, B_const], writes=[B_y])
                            if br_ == 1:
                                kb.dma('sp', yt_, moT.rearrange("(k p) t -> p k t", p=128), reads=[B_scr['moT']], writes=[B_yt])
                                kb.op('dve', lambda e, yb_=yb_, yt_=yt_: e.tensor_tensor(out=yb_[:], in0=yb_[:], in1=yt_, op=ALU.mult), reads=[B_y, B_yt], writes=[B_y])
                        wub = [sb(f"C_wu{i}", [128, max(KU), 512], BF16) for i in range(3)]
                        Bwu = [Buf() for _ in range(3)]
                        gtr = Ring([sb(f"C_gt{i}", [128, 512], BF16) for i in range(4)])
                        acc = Ring([sb(f"C_acc{i}", [128, 512], F32) for i in range(2)])
                        tmpr = Ring([sb(f"C_tmp{i}", [128, 512], F32) for i in range(2)])
                        wi = 0
                        bank = 0
                        for cb in range(D // 512):
                            wsel = []
                            for br_ in range(3):
                                wbuf, bw = wub[wi % 3], Bwu[wi % 3]
                                wi += 1
                                kb.dma('pool', wbuf[:, :KU[br_], :], wview(w_up[br_][l], 0, KU[br_], cb * 512, 512), writes=[bw])
                                wsel.append((wbuf, bw))
                            for cc in range(4):
                                for tg in range(NTG):
                                    ac, bac = acc.next()
                                    chunk = cb * 4 + cc
                                    for br_ in range(3):
                                        ps, pb = psum[bank % 8], PB[bank % 8]
                                        bank += 1
                                        wbuf, bw = wsel[br_]

                                        def mm(e, ps=ps, wbuf=wbuf, br_=br_, cc=cc, tg=tg):
                                            ins = None
                                            for k in range(KU[br_]):
                                                ins = e.matmul(ps[:, :], lhsT=wbuf[:, k, cc * 128:(cc + 1) * 128], rhs=ybuf[br_][:, k, tg * 512:(tg + 1) * 512],
                                                               start=(k == 0), stop=(k == KU[br_] - 1))
                                            return ins
                                        kb.op('pe', mm, reads=[bw, B_y], writes=[pb])
                                        gt, bgt = gtr.next()
                                        r0 = br_ * D + chunk * 128
                                        kb.dma('sp', gt[:], gT[r0:r0 + 128, t0 + tg * 512:t0 + (tg + 1) * 512], reads=[B_scr['gT']], writes=[bgt])
                                        if br_ == 0:
                                            kb.op('dve', lambda e, ac=ac, ps=ps, gt=gt: e.tensor_tensor(out=ac[:], in0=ps[:, :], in1=gt[:], op=ALU.mult),
                                                  reads=[pb, bgt], writes=[bac])
                                        else:
                                            tm, btm = tmpr.next()
                                            kb.op('dve', lambda e, tm=tm, ps=ps, gt=gt: e.tensor_tensor(out=tm[:], in0=ps[:, :], in1=gt[:], op=ALU.mult),
                                                  reads=[pb, bgt], writes=[btm])
                                            if br_ == 1:
                                                kb.op('dve', lambda e, ac=ac, tm=tm: e.tensor_tensor(out=ac[:], in0=ac[:], in1=tm[:], op=ALU.add),
                                                      reads=[btm, bac], writes=[bac])
                                            else:
                                                kb.op('dve', lambda e, ac=ac, tm=tm, chunk=chunk, tg=tg: e.tensor_tensor(out=actA[:, chunk, tg * 512:(tg + 1) * 512], in0=ac[:], in1=tm[:], op=ALU.add),
                                                      reads=[btm, bac], awrites=[B_A])
                        kb.barrier()

                    def gemm_resid(W2d, KTOT, act_loader, res_src, B_res, tagp, B_wsrc=None):
                        with ExitStack() as es_l:
                            nonlocal es2
                            es2 = es_l
                            KP = 16 if KTOT > DC else DC
                            npieces = KTOT // KP
                            wb_ = [sb(f"{tagp}_w{i}", [128, KP, 512], BF16) for i in range(2)]
                            Bw_ = [Buf(), Buf()]
                            xr = Ring([sb(f"{tagp}_xr{i}", [128, 512], F32) for i in range(3)])
                            ro = Ring([sb(f"{tagp}_ro{i}", [128, 512], F32) for i in range(3)])
                            wi = 0
                            for cb in range(D // 512):
                                for kp in range(npieces):
                                    wbuf, bw = wb_[wi % 2], Bw_[wi % 2]
                                    wi += 1
                                    kb.dma('pool', wbuf[:], wview(W2d, kp * KP, KP, cb * 512, 512), reads=([B_wsrc] if B_wsrc is not None else []), writes=[bw])
                                    at, bat, koff = act_loader(kp, KP)
                                    for tt in range(NTT):
                                        ps, pb = psum[tt], PB[tt]

                                        def mm(e, ps=ps, wbuf=wbuf, at=at, koff=koff, tt=tt, kp=kp):
                                            ins = None
                                            for k in range(KP):
                                                ins = e.matmul(ps[:, :], lhsT=at[:, koff + k, tt * 128:(tt + 1) * 128], rhs=wbuf[:, k, :],
                                                               start=(kp == 0 and k == 0), stop=(kp == npieces - 1 and k == KP - 1))
                                            return ins
                                        kb.op('pe', mm, reads=[bw, bat], **({'writes': [pb]} if kp == 0 else {'awrites': [pb]}))
                                        if kp == npieces - 1:
                                            xt_, bx_ = xr.next()
                                            kb.dma('sp', xt_[:], res_src[t0 + tt * 128:t0 + (tt + 1) * 128, cb * 512:(cb + 1) * 512], reads=[B_res], writes=[bx_])
                                            rt_, br__ = ro.next()
                                            kb.op('dve', lambda e, rt_=rt_, xt_=xt_, ps=ps: e.scalar_tensor_tensor(out=rt_[:], in0=xt_[:], scalar=c.alpha, in1=ps[:, :],
                                                                                                                 op0=ALU.mult, op1=ALU.add),
                                                  reads=[pb, bx_], writes=[br__])
                                            kb.dma('sp', r_scr[t0 + tt * 128:t0 + (tt + 1) * 128, cb * 512:(cb + 1) * 512], rt_[:], reads=[br__], awrites=[B_scr['r']])
                            kb.barrier()

                    def ln_pass(g_ap, b_ap, dst, B_dst, tagp, also_xT_scr):
                        with ExitStack() as es_l:
                            nonlocal es2
                            es2 = es_l
                            gam = sb(f"{tagp}_g", [128, D], F32)
                            bet = sb(f"{tagp}_b", [128, D], F32)
                            B_gb = Buf()
                            kb.dma('sp', gam[:], bass.AP(g_ap.tensor, g_ap.offset, [[0, 128], [1, D]]), writes=[B_gb])
                            kb.dma('sp', bet[:], bass.AP(b_ap.tensor, b_ap.offset, [[0, 128], [1, D]]), awrites=[B_gb])
                            NR = 3
                            rt = [sb(f"{tagp}_rt{i}", [128, D], F32) for i in range(NR)]
                            Brt = [Buf() for _ in range(NR)]
                            junk = sb(f"{tagp}_junk", [128, D], BF16)
                            B_junk = Buf()
                            stat = [sb(f"{tagp}_st{i}", [128, 8], F32) for i in range(NR)]
                            Bst = [Buf() for _ in range(NR)]
                            rb = [sb(f"{tagp}_rb{i}", [128, D], BF16) for i in range(NR)]
                            Brb = [Buf() for _ in range(NR)]

                            def lnA1(tt):
                                i = tt % NR
                                r_, s_, bs_ = rt[i], stat[i], Bst[i]
                                rows = slice(t0 + tt * 128, t0 + (tt + 1) * 128)
                                kb.dma('sp', r_[:], r_scr[rows, :], reads=[B_scr['r']], writes=[Brt[i]])
                                kb.op('dve', lambda e: e.memset(s_[:], 0.0), writes=[bs_])
                                kb.op('act', lambda e: e.activation(out=junk[:], in_=r_[:], func=AF.Copy, accum_out=s_[:, 0:1]),
                                      reads=[Brt[i]], writes=[B_junk], awrites=[bs_])
                                kb.op('act', lambda e: e.activation(out=junk[:], in_=r_[:], func=AF.Square, accum_out=s_[:, 1:2]),
                                      reads=[Brt[i]], writes=[B_junk], awrites=[bs_])

                            def lnA2(tt):
                                i = tt % NR
                                r_, s_, bs_ = rt[i], stat[i], Bst[i]
                                kb.op('dve', lambda e: e.tensor_scalar_mul(out=s_[:, 2:3], in0=s_[:, 0:1], scalar1=1.0 / D), reads=[bs_], writes=[bs_])
                                kb.op('dve', lambda e: e.tensor_tensor(out=s_[:, 3:4], in0=s_[:, 2:3], in1=s_[:, 2:3], op=ALU.mult), reads=[bs_], writes=[bs_])
                                kb.op('dve', lambda e: e.scalar_tensor_tensor(out=s_[:, 4:5], in0=s_[:, 1:2], scalar=1.0 / D, in1=s_[:, 3:4], op0=ALU.mult, op1=ALU.subtract),
                                      reads=[bs_], writes=[bs_])
                                kb.op('act', lambda e: e.activation(out=s_[:, 6:7], in_=s_[:, 4:5], func=AF.Sqrt, bias=1e-5, scale=1.0), reads=[bs_], writes=[bs_])
                                kb.op('dve', lambda e: e.reciprocal(out=s_[:, 7:8], in_=s_[:, 6:7]), reads=[bs_], writes=[bs_])
                                kb.op('dve', lambda e: e.scalar_tensor_tensor(out=s_[:, 5:6], in0=s_[:, 2:3], scalar=-1.0, in1=s_[:, 7:8], op0=ALU.mult, op1=ALU.mult),
                                      reads=[bs_], writes=[bs_])
                                kb.op('act', lambda e: e.activation(out=r_[:], in_=r_[:], func=AF.Identity, scale=s_[:, 7:8], bias=s_[:, 5:6]),
                                      reads=[bs_, Brt[i]], writes=[Brt[i]])

                            def lnB1(tt):
                                i = tt % NR
                                r_ = rt[i]
                                rows = slice(t0 + tt * 128, t0 + (tt + 1) * 128)
                                kb.op('dve', lambda e: e.tensor_tensor(out=r_[:], in0=r_[:], in1=gam[:], op=ALU.mult), reads=[Brt[i], B_gb], writes=[Brt[i]])
                                kb.op('dve', lambda e: e.tensor_tensor(out=rb[i][:], in0=r_[:], in1=bet[:], op=ALU.add), reads=[Brt[i], B_gb], writes=[Brb[i]])
                                kb.op('pool', lambda e: e.tensor_tensor(out=r_[:], in0=r_[:], in1=bet[:], op=ALU.add), reads=[Brt[i], B_gb], writes=[Brt[i]])
                                kb.dma('sp', dst[rows, :], r_[:], reads=[Brt[i]], awrites=[B_dst])

                            def lnB2(tt):
                                i = tt % NR
                                transpose_tile(rb[i], Brb[i], lambda cq: actA[:, cq * 4:(cq + 1) * 4, tt * 128:(tt + 1) * 128], B_A, 'awrites')

                            lnA1(0)
                            lnA2(0)
                            for tt in range(NTT):
                                if tt + 1 < NTT:
                                    lnA1(tt + 1)
                                lnB1(tt)
                                if tt + 1 < NTT:
                                    lnA2(tt + 1)
                                lnB2(tt)
                            kb.barrier()
                            if also_xT_scr:
                                kb.dma('sp', xT_v[:, :, t0:t0 + TH], actA[:], reads=[B_A], awrites=[B_xT])
                                kb.barrier()

                    gemm_resid(w_o[l], DC, lambda kp, KP: (actA, B_A, 0), x_res, B_xres, "Wo")
                    B_A = Buf()
                    ln_pass(ln1_g[l], ln1_b[l], x1_scr, B_scr['x1'], "L1", False)
                    with ExitStack() as es_l:
                        es2 = es_l
                        wbufs = [sb(f"F1_w{i}", [128, DC, 512], BF16) for i in range(2)]
                        Bw = [Buf(), Buf()]
                        wctr = [0]
                        rl_ = Ring([sb(f"F1_r{i}", [128, 512], F32) for i in range(3)])
                        hs = Ring([sb(f"F1_h{i}", [128, 512], BF16) for i in range(4)])
                        blocks = []
                        for c0 in range(0, DFF, 512):
                            def wload(wb, c0=c0):
                                return [(wb[:, :, :], wview(w_ff1[l], 0, DC, c0, 512))]

                            def epi(cc, tg, ps, pb, c0=c0):
                                rr, brr = rl_.next()
                                kb.op('act', lambda e: e.activation(out=rr[:], in_=ps[:, :], func=AF.Relu), reads=[pb], writes=[brr])
                                hh, bhh = hs.next()
                                kb.op('dve', lambda e: e.tensor_tensor(out=hh[:], in0=rr[:], in1=rr[:], op=ALU.mult), reads=[brr], writes=[bhh])
                                r0 = c0 + cc * 128
                                kb.dma('sp', hidT[r0:r0 + 128, tg * 512:(tg + 1) * 512], hh[:], reads=[bhh], awrites=[B_scr['hid']])
                            blocks.append(dict(wload=wload, ncols=512, orient='F', epi=epi))
                        gemm_half(actA, B_A, DC, blocks, wbufs, Bw, wctr)
                        kb.barrier()
                    with ExitStack() as es_l:
                        es2 = es_l
                        hb = [sb(f"F2_h{i}", [128, 16, TH], BF16) for i in range(2)]
                        Bhb = [Buf(), Buf()]
                        hctr = [0]
                        hid_v = hidT.rearrange("(k p) t -> p k t", p=128)

                        def hid_loader(kp, KP):
                            i = hctr[0] % 2
                            hctr[0] += 1
                            kb.dma('act', hb[i][:], hid_v[:, kp * KP:(kp + 1) * KP, :], reads=[B_scr['hid']], writes=[Bhb[i]])
                            return hb[i], Bhb[i], 0
                        gemm_resid(w_ff2[l], DFF // 128, hid_loader, x1_scr, B_scr['x1'], "F2")
                    B_A = Buf()
                    B_scr['hid'] = Buf()
                    ln_pass(ln2_g[l], ln2_b[l], x_dst, B_xdst, "L2", l < L - 1)
            x_res = x2_scr
            B_xres = B_xdst
        kb.final_wait()
    return nc


def _consts(cfg):
    ident = np.eye(128, dtype=np.float32)
    tri = np.triu(np.ones((128, 128), dtype=np.float32))
    slopes = 2.0 ** (-8.0 * np.arange(1, cfg.HA + 1, dtype=np.float64) / cfg.HA)
    k = np.arange(128)[:, None].astype(np.float64)
    q = np.arange(128)[None, :].astype(np.float64)
    E = np.zeros((128, cfg.HA, 256), dtype=np.float32)
    for h in range(cfg.HA):
        dprev = q + 128 - k
        E[:, h, 0:128] = np.where(dprev < 128, np.exp(-slopes[h] * dprev), 0.0)
        dcur = q - k
        E[:, h, 128:256] = np.where(dcur >= 0, np.exp(-slopes[h] * dcur), 0.0)
    return ident, tri, E.reshape(128, cfg.HA * 256)


_NAMES = ['b_mlstm_i', 'b_mlstm_f', 'b_fox_f', 'attn_sinks', 'w_up_swa', 'w_up_mlstm', 'w_up_fox', 'w_o',
          'ln1_g', 'ln1_b', 'ln2_g', 'ln2_b']
_SPLIT = ['w_in', 'w_ff1', 'w_ff2']


def run(cfg, inputs, debug=False):
    nc = build_program(cfg, debug=debug)
    ident, tri, E = _consts(cfg)
    x = np.asarray(inputs['x'], dtype=np.float32)
    B = x.shape[0]
    NCr = cfg.NCORES
    assert 2 * B == NCr
    TH = cfg.TH
    shared = {n: np.ascontiguousarray(np.asarray(inputs[n], dtype=np.float32)) for n in _NAMES}
    for n in _SPLIT:
        a = np.asarray(inputs[n], dtype=np.float32)
        for i in range(cfg.L):
            shared[f"{n}_{i}"] = np.ascontiguousarray(a[i])
    in_maps = []
    for cidx in range(NCr):
        b, p = cidx // 2, cidx % 2
        m = dict(shared)
        m['x'] = np.ascontiguousarray(x[b, p * TH:(p + 1) * TH])
        m['flags'] = np.array([[1.0 - p, float(p)]], dtype=np.float32)
        m['c_ident'] = ident
        m['c_tri'] = tri
        m['c_alibi'] = E
        in_maps.append(m)
    res = run_bass_kernel_spmd(nc, in_maps, core_ids=list(range(NCr)))
    if debug:
        return res.results
    out = np.empty((B, cfg.T, cfg.D), dtype=np.float32)
    for cidx in range(NCr):
        b, p = cidx // 2, cidx % 2
        out[b, p * TH:(p + 1) * TH] = res.results[cidx]['y']
    return out


def kernel(**inputs):
    cfg = Cfg()
    return run(cfg, inputs)
```
